# Optimizing a Trainium2 kernel written in Bass

```python
import jax, jax.numpy as jnp
from jax import lax
import numpy as np

D_MODEL = 4096
BATCH = 2
SEQ = 8192
DEPTH = 1

HEAD_DIM = 128
N_RET_HEADS = D_MODEL // (2 * HEAD_DIM)
N_SB_HEADS = D_MODEL // (2 * HEAD_DIM)
RET_WIDTH = N_RET_HEADS * HEAD_DIM
SB_WIDTH = N_SB_HEADS * HEAD_DIM
MIX_WIDTH = RET_WIDTH + SB_WIDTH
IN_SPLITS = [RET_WIDTH] * 4 + [SB_WIDTH] * 3
IN_WIDTH = sum(IN_SPLITS)
RET_CHUNK = 128
SB_BLOCK = 128
N_MEM = 256
N_CROSS_HEADS = 4
CROSS_WIDTH = N_CROSS_HEADS * HEAD_DIM
D_FF = 11008
CONV_WIDTH = 3
ROPE_BASE = 10000.0
EPS = 1e-6

kernel_name = "hybrid_retention_stickbreaking_layer"


def rmsnorm(x, g):
    xf = x.astype(jnp.float32)
    y = xf * lax.rsqrt(jnp.mean(xf * xf, axis=-1, keepdims=True) + EPS)
    return (y * g.astype(jnp.float32)).astype(x.dtype)


def rope_tables(S):
    inv_freq = ROPE_BASE ** (-jnp.linspace(0.0, 1.0, HEAD_DIM // 2, dtype=jnp.float32))
    ang = jnp.arange(S, dtype=jnp.float32)[:, None] * inv_freq[None, :]
    return jnp.cos(ang), jnp.sin(ang)


def apply_rope(x, cos, sin):
    c = cos[None, :, None, :].astype(x.dtype)
    s = sin[None, :, None, :].astype(x.dtype)
    x1, x2 = jnp.split(x, 2, axis=-1)
    return jnp.concatenate([x1 * c - x2 * s, x2 * c + x1 * s], axis=-1)


def retention_chunkwise(q, k, v):
    B, H, S, d = q.shape
    C = RET_CHUNK
    N = S // C
    dt = q.dtype
    log_g = jnp.log1p(-jnp.exp2(-5.0 - jnp.arange(H, dtype=jnp.float32)))
    idx = jnp.arange(C, dtype=jnp.float32)
    diff = idx[:, None] - idx[None, :]
    intra_decay = jnp.where(diff >= 0, jnp.exp(jnp.maximum(diff, 0.0)[None] * log_g[:, None, None]), 0.0)
    qc = q.reshape(B, H, N, C, d)
    kc = k.reshape(B, H, N, C, d)
    vc = v.reshape(B, H, N, C, d)
    scores = jnp.einsum('bhncd,bhnmd->bhncm', qc, kc) * intra_decay[:, None].astype(dt)
    intra = jnp.einsum('bhncm,bhnmd->bhncd', scores, vc)
    k_decay = jnp.exp((C - 1 - idx)[None, :] * log_g[:, None])[:, None, :, None].astype(dt)
    chunk_kv = jnp.einsum('bhncd,bhnce->nbhde', kc * k_decay, vc)
    chunk_decay = jnp.exp(C * log_g)[None, :, None, None].astype(dt)

    def step(state, kv):
        return state * chunk_decay + kv, state

    _, prev_state = lax.scan(step, jnp.zeros((B, H, d, d), dt), chunk_kv)
    q_decay = jnp.exp((idx + 1.0)[None, :] * log_g[:, None])[:, None, :, None].astype(dt)
    cross = jnp.einsum('bhncd,nbhde->bhnce', qc * q_decay, prev_state)
    return (intra + cross).reshape(B, H, S, d)


def stick_breaking_attention(q, k, v):
    B, H, S, d = q.shape
    Q = SB_BLOCK
    N = S // Q
    scale = HEAD_DIM ** -0.5
    q_blocks = q.reshape(B, H, N, Q, d).transpose(2, 0, 1, 3, 4)
    starts = jnp.arange(N, dtype=jnp.int32) * Q
    key_pos = jnp.arange(S, dtype=jnp.int32)

    def block(args):
        qb, t0 = args
        z = jnp.einsum('bhqd,bhkd->bhqk', qb, k).astype(jnp.float32) * scale
        q_pos = t0 + jnp.arange(Q, dtype=jnp.int32)
        mask = key_pos[None, :] < q_pos[:, None]
        log_stay = jnp.where(mask, jax.nn.log_sigmoid(-z), 0.0)
        log_a = jax.nn.log_sigmoid(z) + lax.cumsum(log_stay, axis=3, reverse=True) - log_stay
        a = jnp.where(mask, jnp.exp(log_a), 0.0).astype(v.dtype)
        return jnp.einsum('bhqk,bhkd->bhqd', a, v)

    out = lax.map(block, (q_blocks, starts))
    return out.transpose(1, 2, 0, 3, 4).reshape(B, H, S, d)


def hybrid_mixer(xn, w_in, ret_norm, sb_q_norm, sb_k_norm, sb_out_norm, w_out):
    B, S, _ = xn.shape
    proj = xn @ w_in
    offsets = [int(o) for o in np.cumsum(IN_SPLITS)[:-1]]
    rq, rk, rv, rg, sq, sk, sv = jnp.split(proj, offsets, axis=-1)

    def heads(t, h):
        return t.reshape(B, S, h, HEAD_DIM)

    cos, sin = rope_tables(S)
    rq = apply_rope(heads(rq, N_RET_HEADS), cos, sin) * (HEAD_DIM ** -0.5)
    rk = apply_rope(heads(rk, N_RET_HEADS), cos, sin)
    rv = heads(rv, N_RET_HEADS)
    ret = retention_chunkwise(rq.transpose(0, 2, 1, 3), rk.transpose(0, 2, 1, 3), rv.transpose(0, 2, 1, 3))
    ret = rmsnorm(ret.transpose(0, 2, 1, 3), ret_norm).reshape(B, S, RET_WIDTH)
    ret = ret * jax.nn.silu(rg)

    sq = rmsnorm(heads(sq, N_SB_HEADS), sb_q_norm)
    sk = rmsnorm(heads(sk, N_SB_HEADS), sb_k_norm)
    sv = heads(sv, N_SB_HEADS)
    sb = stick_breaking_attention(sq.transpose(0, 2, 1, 3), sk.transpose(0, 2, 1, 3), sv.transpose(0, 2, 1, 3))
    sb = rmsnorm(sb.transpose(0, 2, 1, 3), sb_out_norm).reshape(B, S, SB_WIDTH)

    return jnp.concatenate([ret, sb], axis=-1) @ w_out


def memory_cross_attention(xn, memn, w_q, w_kv, q_norm, k_norm, w_o):
    B, S, _ = xn.shape
    M = memn.shape[1]
    q = rmsnorm((xn @ w_q).reshape(B, S, N_CROSS_HEADS, HEAD_DIM), q_norm)
    k, v = jnp.split(memn @ w_kv, 2, axis=-1)
    k = rmsnorm(k.reshape(B, M, N_CROSS_HEADS, HEAD_DIM), k_norm)
    v = v.reshape(B, M, N_CROSS_HEADS, HEAD_DIM)
    s = jnp.einsum('bshd,bmhd->bhsm', q, k).astype(jnp.float32) * (HEAD_DIM ** -0.5)
    p = jax.nn.softmax(s, axis=-1).astype(v.dtype)
    o = jnp.einsum('bhsm,bmhd->bshd', p, v).reshape(B, S, CROSS_WIDTH)
    return o @ w_o


def conv_ffn(xn, w_up, conv_w, conv_b, w_down):
    S = xn.shape[1]
    u = xn @ w_up
    up = jnp.pad(u, ((0, 0), (CONV_WIDTH - 1, 0), (0, 0)))
    c = conv_b
    for j in range(CONV_WIDTH):
        c = c + up[:, j:j + S] * conv_w[j]
    g, val = jnp.split(c, 2, axis=-1)
    return (jax.nn.silu(g) * val) @ w_down


def setup_inputs(seed: int = 0) -> dict:
    key = jax.random.key(seed)
    ks = jax.random.split(key, 21)
    f32 = jnp.float32

    def nrm(k, shape, scale):
        return jax.random.normal(k, shape, f32) * scale

    def gain(k, n):
        return 1.0 + 0.02 * jax.random.normal(k, (DEPTH, n), f32)

    return {
        "x": nrm(ks[0], (BATCH, SEQ, D_MODEL), 1.0),
        "mem": nrm(ks[1], (BATCH, N_MEM, D_MODEL), 1.0),
        "attn_norm": gain(ks[2], D_MODEL),
        "w_in": nrm(ks[3], (DEPTH, D_MODEL, IN_WIDTH), D_MODEL ** -0.5),
        "ret_norm": gain(ks[4], HEAD_DIM),
        "sb_q_norm": gain(ks[5], HEAD_DIM),
        "sb_k_norm": gain(ks[6], HEAD_DIM),
        "sb_out_norm": gain(ks[7], HEAD_DIM),
        "w_out": nrm(ks[8], (DEPTH, MIX_WIDTH, D_MODEL), MIX_WIDTH ** -0.5),
        "cross_norm": gain(ks[9], D_MODEL),
        "mem_norm": gain(ks[10], D_MODEL),
        "cross_w_q": nrm(ks[11], (DEPTH, D_MODEL, CROSS_WIDTH), D_MODEL ** -0.5),
        "cross_w_kv": nrm(ks[12], (DEPTH, D_MODEL, 2 * CROSS_WIDTH), D_MODEL ** -0.5),
        "cross_q_norm": gain(ks[13], HEAD_DIM),
        "cross_k_norm": gain(ks[14], HEAD_DIM),
        "cross_w_o": nrm(ks[15], (DEPTH, CROSS_WIDTH, D_MODEL), CROSS_WIDTH ** -0.5),
        "ffn_norm": gain(ks[16], D_MODEL),
        "ffn_w_up": nrm(ks[17], (DEPTH, D_MODEL, 2 * D_FF), D_MODEL ** -0.5),
        "ffn_conv_w": nrm(ks[18], (DEPTH, CONV_WIDTH, 2 * D_FF), CONV_WIDTH ** -0.5),
        "ffn_conv_b": nrm(ks[19], (DEPTH, 2 * D_FF), 0.01),
        "ffn_w_down": nrm(ks[20], (DEPTH, D_FF, D_MODEL), D_FF ** -0.5),
    }


def reference(x, mem, attn_norm, w_in, ret_norm, sb_q_norm, sb_k_norm, sb_out_norm, w_out,
              cross_norm, mem_norm, cross_w_q, cross_w_kv, cross_q_norm, cross_k_norm, cross_w_o,
              ffn_norm, ffn_w_up, ffn_conv_w, ffn_conv_b, ffn_w_down):
    h = x
    for l in range(DEPTH):
        h = h + hybrid_mixer(rmsnorm(h, attn_norm[l]), w_in[l], ret_norm[l], sb_q_norm[l],
                             sb_k_norm[l], sb_out_norm[l], w_out[l])
        h = h + memory_cross_attention(rmsnorm(h, cross_norm[l]), rmsnorm(mem, mem_norm[l]),
                                       cross_w_q[l], cross_w_kv[l], cross_q_norm[l],
                                       cross_k_norm[l], cross_w_o[l])
        h = h + conv_ffn(rmsnorm(h, ffn_norm[l]), ffn_w_up[l], ffn_conv_w[l], ffn_conv_b[l],
                         ffn_w_down[l])
    return h
```

```python
import numpy as np
import ml_dtypes
import concourse.bass as bass
import concourse.mybir as mybir
from concourse.bass_utils import run_bass_kernel_spmd

AF = mybir.ActivationFunctionType
ALU = mybir.AluOpType
AX = mybir.AxisListType
F32, BF, F16 = mybir.dt.float32, mybir.dt.bfloat16, mybir.dt.float16

D = 4096
S = 8192
NH = 16
DFF = 11008
NFC = 86
EPS = 1e-6
OWN0 = 6144
HALO0 = 6016
OWNROWS = 2176
SCR0 = 5632
SCALE = 128 ** -0.5


class Eng:
    def __init__(self, nc, e, name):
        self.e = e
        self.sem = nc.alloc_semaphore("sem_" + name)
        self.cnt = 0
        self.seen = {}
        self.name = name

    def wait(self, *evs):
        for ev in evs:
            if ev is None:
                continue
            sem, val = ev
            k = id(sem)
            if self.seen.get(k, -1) >= val:
                continue
            self.seen[k] = val
            self.e.wait_ge(sem, val)

    def done(self, ins):
        self.cnt += 1
        ins.then_inc(self.sem, 1)
        return (self.sem, self.cnt)


class Slot:
    def __init__(self, nc, name):
        self.sem = nc.alloc_semaphore("dsem_" + name)
        self.cnt = 0

    def inc(self, ins, n=1):
        self.cnt += 16
        ins.then_inc(self.sem, 16)
        return (self.sem, self.cnt)


class _Stop(Exception):
    pass


def build_program(stop_after=None, debug=False, trace=None):
    nc = bass.Bass("TRN2", target_bir_lowering=False)
    rec_trace = []
    skind = "ExternalOutput" if debug else "Internal"

    def checkpoint(name):
        if stop_after is not None and name == stop_after:
            raise _Stop()

    def din(name, shape, dt=F32):
        return nc.dram_tensor(name, list(shape), dt, kind="ExternalInput").ap()

    x = din("x", [S, D])
    mem = din("mem", [256, D])
    w_in = din("w_in", [56, 128, 8192])
    w_out = din("w_out", [16, 128, 8192])
    w_cq = din("w_cq", [2, 128, 8192])
    w_ckv = din("w_ckv", [4, 128, 8192])
    w_co = din("w_co", [16, 128, 1024])
    w_up = din("w_up", [NFC, 128, 8192])
    w_dn0 = din("w_dn0", [16, 128, 8192])
    w_dn1 = din("w_dn1", [16, 128, 8192])
    w_dn2 = din("w_dn2", [16, 128, 22 * 256])
    gcols = din("gcols", [128, 4 * 32])
    hg = din("hg", [128, 8])
    cw = din("cw", [128, NFC * 2 * 4])
    rtab = din("rtab", [128, 3 * NH])
    qrow = din("qrow", [128, NH * 128])
    cmats = din("cmats", [128, 5 * 128], BF)
    hmats = din("hmats", [128, 2 * 128], F16)
    ropec = din("ropec", [128, 64 * 64])
    ropes = din("ropes", [128, 64 * 64])
    ropen = din("ropen", [128, 64 * 64])
    flag = din("flag", [128, 1])
    y = nc.dram_tensor("y", [2048, D], F32, kind="ExternalOutput").ap()

    KT = nc.dram_tensor("KT", [NH, 128, S], BF, kind=skind).ap()
    VS = nc.dram_tensor("VS", [S, 2048], BF, kind=skind).ap()
    RKo = nc.dram_tensor("RKo", [S - SCR0, 2048], BF, kind=skind).ap()
    RVo = nc.dram_tensor("RVo", [S - SCR0, 2048], BF, kind=skind).ap()
    STs = nc.dram_tensor("STs", [17, 128, 2048], BF, kind=skind).ap()
    H1 = nc.dram_tensor("H1", [OWNROWS, D], F32, kind=skind).ap()
    H2 = nc.dram_tensor("H2", [OWNROWS, D], F32, kind=skind).ap()

    PE = Eng(nc, nc.tensor, "pe")
    ACT = Eng(nc, nc.scalar, "act")
    DVE = Eng(nc, nc.vector, "dve")
    POOL = Eng(nc, nc.gpsimd, "pool")
    SP = Eng(nc, nc.sync, "sp")
    ENGS = [PE, ACT, DVE, POOL, SP]
    pending_dma = []

    def dma(q, slot, out, in_):
        ev = slot.inc(q.e.dma_start(out=out, in_=in_))
        pending_dma.append(ev)
        return ev

    def barrier():
        mx = {}
        for (sm, v) in pending_dma:
            if id(sm) not in mx or mx[id(sm)][1] < v:
                mx[id(sm)] = (sm, v)
        evs = [(e.sem, e.cnt) for e in ENGS if e.cnt > 0] + list(mx.values())
        for e in ENGS:
            e.wait(*evs)
        pending_dma.clear()

    def sb(name, shape, dt):
        return nc.alloc_sbuf_tensor("sb_" + name, list(shape), dt)

    slabs = [sb(f"slab{i}", [128, 8192], BF) for i in range(2)]
    slab_slot = [Slot(nc, f"slab{i}") for i in range(2)]
    slab_free = [None, None]
    xnT_t = sb("xnT", [128, 32, 512], BF)
    xs = sb("xs", [128, 4096], F32)
    xb = sb("xb", [128, 4096], BF)
    junk = xb
    big = sb("big", [128, NFC * 512], BF)
    gcols_t = sb("gcols", [128, 128], F32)
    hg_t = sb("hg", [128, 8], F32)
    cw_t = sb("cw", [128, NFC * 8], F32)
    rtab_t = sb("rtab", [128, 48], F32)
    cm_t = sb("cm", [128, 640], BF)
    hm_t = sb("hm", [128, 256], F16)
    dmask_f = sb("dmaskf", [128, 128], F32)
    dmask_h = sb("dmaskh", [128, 128], F16)
    mask01_f = sb("mask01f", [128, 128], F32)
    carry = sb("carry", [128, NFC * 2, 2], F32)
    state = sb("state", [128, NH, 128], F32)
    ss = sb("ss", [128, 8], F32)
    kmT = sb("kmT", [128, 4, 256], BF)
    vm = sb("vm", [128, 2, 512], BF)
    rp_c = sb("rp_c", [128, 4, 64], F32)
    rp_s = sb("rp_s", [128, 4, 64], F32)
    rp_n = sb("rp_n", [128, 4, 64], F32)
    qrow_t = sb("qrow_t", [128, 2, 128], F32)
    flag_t = sb("flag_t", [128, 1], F32)
    cst_t = sb("cst_t", [128, 4], F32)

    ident = cm_t[:, 0:128]
    ones = cm_t[:, 128:256]
    Umat = hm_t[:, 0:128]
    Lmat = hm_t[:, 128:256]

    mixT = big[:, 0:32 * 512].rearrange("p (c t) -> p c t", t=512)
    aT = big[:, :].rearrange("p (c t) -> p c t", t=512)
    SCR_OFF = 32 * 512

    class Arena:
        def __init__(self):
            self.off = SCR_OFF

        def reset(self):
            self.off = SCR_OFF

        def get(self, n, dt):
            mult = 2 if dt == F32 else 1
            self.off = (self.off + 15) // 16 * 16
            a = big[:, self.off:self.off + n * mult]
            self.off += n * mult
            assert self.off <= NFC * 512, "arena overflow"
            if dt == F32:
                return a.bitcast(F32)
            if dt == F16:
                return a.bitcast(F16)
            return a
    AR = Arena()

    pairs = [nc.alloc_psum_tensor(f"pp{i}", [128, 1024], F32) for i in range(4)]
    banks = [pairs[i // 2][:, (i % 2) * 512:(i % 2 + 1) * 512] for i in range(8)]
    bank_free = [None] * 8
    bank_rr = [0]

    def get_bank(choices=(0, 1, 2, 3, 4, 5)):
        i = choices[bank_rr[0] % len(choices)]
        bank_rr[0] += 1
        return i

    sl_const = Slot(nc, "const")
    sl_xs = Slot(nc, "xs")
    sl_xs2 = [Slot(nc, "xsa"), Slot(nc, "xsb")]
    sl_misc = Slot(nc, "misc")
    sl_out = Slot(nc, "out")
    sl_k = [Slot(nc, "k0"), Slot(nc, "k1")]
    sl_kv = [Slot(nc, "kv0"), Slot(nc, "kv1"), Slot(nc, "kv2")]
    sl_st = Slot(nc, "st")
    sl_kst = [Slot(nc, "kst0"), Slot(nc, "kst1")]
    sl_vst = [Slot(nc, "vst0"), Slot(nc, "vst1")]
    sl_rk = Slot(nc, "rk")
    sl_rv = Slot(nc, "rv")
    sl_rope = Slot(nc, "rope")
    sl_rld = [Slot(nc, "rld0"), Slot(nc, "rld1")]
    sl_rst = [Slot(nc, "rst0"), Slot(nc, "rst1")]
    sl_dbg = Slot(nc, "dbg")

    cev = []
    for dst, src in ((gcols_t, gcols), (hg_t, hg), (cw_t, cw), (rtab_t, rtab), (cm_t, cmats), (hm_t, hmats), (flag_t, flag)):
        cev.append(dma(SP, sl_const, dst[:], src))
    for e in (ACT, DVE, PE, POOL):
        e.wait(cev[-1])
    DVE.e.tensor_copy(out=dmask_f[:], in_=cm_t[:, 384:512])
    DVE.e.tensor_copy(out=dmask_h[:], in_=cm_t[:, 384:512])
    DVE.e.tensor_copy(out=mask01_f[:], in_=cm_t[:, 256:384])
    DVE.e.memset(carry[:], 0.0)
    DVE.e.memset(cst_t[:, 0:1], EPS)
    DVE.e.memset(cst_t[:, 1:2], 1.0)
    DVE.done(DVE.e.memset(state[:], 0.0))
    barrier()

    slab_idx = [0]

    WB = {}
    for nm, ns, ncl in (("w_in", 56, 8192), ("w_out", 16, 8192), ("w_cq", 2, 8192), ("w_co", 16, 1024),
                        ("w_up", NFC, 8192), ("w_dn0", 16, 8192), ("w_dn1", 16, 8192), ("w_dn2", 16, 22 * 256)):
        WB[nm] = nc.dram_tensor("wb_" + nm, [ns, 128, ncl], BF).ap()
    converted = {}
    wb_slot = [Slot(nc, "wb0"), Slot(nc, "wb1")]
    slab_slot_hw = [Slot(nc, "slabhw0"), Slot(nc, "slabhw1")]
    wb_pending = [None, None]

    def load_slab(src_ap, ncols):
        i = slab_idx[0] % 2
        slab_idx[0] += 1
        nm = src_ap.name
        key = (nm, src_ap.offset)
        if nm in WB and key in converted:
            wb_ap, wev = converted[key]
            SP.wait(slab_free[i], wb_pending[i], wev)
            ev = dma(SP, slab_slot_hw[i], slabs[i][:, 0:ncols], wb_ap)
            return i, ev
        POOL.wait(slab_free[i], wb_pending[i])
        ev = dma(POOL, slab_slot[i], slabs[i][:, 0:ncols], src_ap)
        if nm in WB:
            idx = src_ap.offset // (128 * ncols)
            wb_ap = WB[nm][idx]
            SP.wait(ev)
            wev = dma(SP, wb_slot[i], wb_ap, slabs[i][:, 0:ncols])
            converted[key] = (wb_ap, wev)
            wb_pending[i] = wev
        return i, ev

    def slab_view(i, kc):
        return slabs[i][:, 0:kc * 256].rearrange("p (k c) -> p k c", c=256)

    WSRC = {"w_in": w_in, "w_out": w_out, "w_cq": w_cq, "w_ckv": w_ckv, "w_co": w_co, "w_up": w_up,
            "w_dn0": w_dn0, "w_dn1": w_dn1, "w_dn2": w_dn2}
    cursor = [0]
    loaded = {}

    def ensure_loaded(g):
        if trace is None or g >= len(trace) or g in loaded:
            return
        nm, off, ncols = trace[g]
        loaded[g] = load_slab(WSRC[nm][off // (128 * ncols)], ncols)

    def stream_slabs(srcs, kcs, body):
        if trace is None:
            nxt = load_slab(srcs[0], kcs[0] * 256)
        for j in range(len(srcs)):
            rec_trace.append((srcs[j].name, srcs[j].offset, kcs[j] * 256))
            if trace is None:
                cur = nxt
                if j + 1 < len(srcs):
                    nxt = load_slab(srcs[j + 1], kcs[j + 1] * 256)
            else:
                g = cursor[0]
                cursor[0] += 1
                assert trace[g] == (srcs[j].name, srcs[j].offset, kcs[j] * 256), "slab trace mismatch"
                ensure_loaded(g)
                ensure_loaded(g + 1)
                cur = loaded.pop(g)
            i, ev = cur
            PE.wait(ev)
            last = body(j, slab_view(i, kcs[j]))
            slab_free[i] = last

    def mm_acc(bank_i, out_ap, pairs, extra_wait=()):
        PE.wait(bank_free[bank_i], *extra_wait)
        n = len(pairs)
        ins = None
        for k, (l, r) in enumerate(pairs):
            ins = PE.e.matmul(out_ap, l, r, start=(k == 0), stop=(k == n - 1))
        return PE.done(ins)

    xnT_free = [None]

    def norm_T(src_rows, nblk, gsel):
        AR.reset()
        xs_b = [xs[:, :], AR.get(4096, F32)]
        xb_b = [xb[:, :], AR.get(4096, BF)]
        xs_free = [None, None]
        xb_free = [None, None]
        for blk in range(nblk):
            k = blk % 2
            c3 = 3 * k
            SP.wait(xs_free[k])
            d = dma(SP, sl_xs2[k], xs_b[k], src_rows(blk))
            ACT.wait(d, xb_free[k])
            a = ACT.done(ACT.e.activation(out=xb_b[k], in_=xs_b[k], func=AF.Square, scale=1.0 / 64.0, accum_out=ss[:, c3:c3 + 1]))
            ACT.wait(a)
            b1 = ACT.done(ACT.e.activation(out=ss[:, c3 + 1:c3 + 2], in_=ss[:, c3:c3 + 1], func=AF.Ln, bias=cst_t[:, 0:1]))
            ACT.wait(b1)
            e1 = ACT.done(ACT.e.activation(out=ss[:, c3 + 2:c3 + 3], in_=ss[:, c3 + 1:c3 + 2], func=AF.Exp, scale=-0.5))
            DVE.wait(e1, xb_free[k])
            e2 = DVE.done(DVE.e.tensor_scalar(out=xb_b[k], in0=xs_b[k], scalar1=ss[:, c3 + 2:c3 + 3], scalar2=None, op0=ALU.mult))
            xs_free[k] = e2
            tr_last = None
            for grp in range(4):
                bi = 6 + (grp % 2)
                pt = banks[bi].bitcast(BF).rearrange("p (a b) -> p a b", b=128)
                PE.wait(e2, bank_free[bi])
                ins = None
                for kk in range(8):
                    c = grp * 8 + kk
                    ins = PE.e.transpose(pt[:, kk, :], xb_b[k][:, c * 128:(c + 1) * 128], ident)
                te = PE.done(ins)
                tr_last = te
                DVE.wait(te, xnT_free[0])
                gb = gcols_t[:, gsel * 32 + grp * 8: gsel * 32 + grp * 8 + 8].unsqueeze(2).to_broadcast([128, 8, 128])
                ee = DVE.done(DVE.e.tensor_tensor(out=xnT_t[:, grp * 8:grp * 8 + 8, blk * 128:(blk + 1) * 128],
                                                  in0=pt[:, 0:8, :], in1=gb, op=ALU.mult))
                bank_free[bi] = ee
            xb_free[k] = tr_last
        barrier()

    def head_norm(src_ap, ntok, gain_ap, out_ap, src_ev, src_is_psum):
        kf = AR.get(ntok, F32)
        sq = AR.get(ntok, BF)
        rs = AR.get(ntok, F32)
        if src_is_psum:
            DVE.wait(src_ev)
            e_kf = DVE.done(DVE.e.tensor_copy(out=kf, in_=src_ap))
            srcf = kf
        else:
            e_kf = src_ev
            srcf = src_ap
        ACT.wait(e_kf)
        e_sq = ACT.done(ACT.e.activation(out=sq, in_=srcf, func=AF.Square))
        b2 = get_bank()
        e_mm = mm_acc(b2, banks[b2][:, 0:ntok], [(ones, sq)], extra_wait=(e_sq,))
        ACT.wait(e_mm)
        r1 = ACT.done(ACT.e.activation(out=rs, in_=banks[b2][:, 0:ntok], func=AF.Ln, bias=cst_t[:, 0:1]))
        bank_free[b2] = r1
        ACT.wait(r1)
        e_r = ACT.done(ACT.e.activation(out=rs, in_=rs, func=AF.Exp, scale=-0.5))
        DVE.wait(e_r, e_kf)
        e_o = DVE.done(DVE.e.scalar_tensor_tensor(out=out_ap, in0=srcf, scalar=gain_ap, in1=rs, op0=ALU.mult, op1=ALU.mult))
        DVE.wait(e_o)
        ACT.wait(e_o)
        return e_o, e_sq, e_kf

    def rope_tok(ps_ap, blk4, out_f32, ev_ps):
        t1 = AR.get(256, F32)
        t2 = AR.get(256, F32)
        p4 = ps_ap.rearrange("p (h two d) -> p h two d", h=2, two=2)
        t14 = t1.rearrange("p (h two d) -> p h two d", h=2, two=2)
        t24 = t2.rearrange("p (h two d) -> p h two d", h=2, two=2)
        cb = rp_c[:, blk4, :].unsqueeze(1).unsqueeze(1).to_broadcast([128, 2, 2, 64])
        sbb = rp_s[:, blk4, :].unsqueeze(1).to_broadcast([128, 2, 64])
        nbb = rp_n[:, blk4, :].unsqueeze(1).to_broadcast([128, 2, 64])
        DVE.wait(ev_ps)
        DVE.e.tensor_tensor(out=t14, in0=p4, in1=cb, op=ALU.mult)
        DVE.e.tensor_tensor(out=t24[:, :, 0, :], in0=p4[:, :, 1, :], in1=nbb, op=ALU.mult)
        e = DVE.done(DVE.e.tensor_tensor(out=t24[:, :, 1, :], in0=p4[:, :, 0, :], in1=sbb, op=ALU.mult))
        DVE.wait(e)
        e2 = DVE.done(DVE.e.tensor_tensor(out=out_f32, in0=t1, in1=t2, op=ALU.add))
        return e2, e

    def load_rope(vt_blk0, nblk):
        for t, src in ((rp_c, ropec), (rp_s, ropes), (rp_n, ropen)):
            s3 = src.rearrange("p (b d) -> p b d", d=64)
            ev = dma(SP, sl_rope, t[:, 0:nblk, :], s3[:, vt_blk0:vt_blk0 + nblk, :])
        return ev

    def mem_kv():
        norm_T(lambda blk: mem[blk * 128:(blk + 1) * 128, :], 2, 3)
        checkpoint("memnorm")
        AR.reset()
        srcs = [w_ckv[i] for i in range(4)]

        def body(j, sv):
            last = None
            if j < 2:
                for hh in range(2):
                    h = 2 * j + hh
                    b = get_bank()
                    e = mm_acc(b, banks[b][:, 0:256], [(sv[:, kc, hh * 128:(hh + 1) * 128], xnT_t[:, kc, 0:256]) for kc in range(32)])
                    last = e
                    checkpoint("mk_mm")
                    eo, esq, ekf = head_norm(banks[b][:, 0:256], 256, hg_t[:, 5:6], kmT[:, h, :], e, True)
                    bank_free[b] = ekf
                    DVE.wait(esq)
                    checkpoint("mk_hn")
            else:
                for blk in range(2):
                    b = get_bank()
                    e = mm_acc(b, banks[b][:, 0:256], [(xnT_t[:, kc, blk * 128:(blk + 1) * 128], sv[:, kc, :]) for kc in range(32)])
                    last = e
                    ACT.wait(e)
                    bank_free[b] = ACT.done(ACT.e.copy(out=vm[:, blk, (j - 2) * 256:(j - 1) * 256], in_=banks[b][:, 0:256]))
            return last
        stream_slabs(srcs, [32] * 4, body)
        xnT_free[0] = (PE.sem, PE.cnt)
        barrier()

    def kv_side(vt):
        AR.reset()
        ev_rope = load_rope(vt * 4, 4)
        DVE.wait(ev_rope)
        kst = [AR.get(512, BF), AR.get(512, BF)]
        kst_free = [None, None]
        vst = [AR.get(4 * 256, BF), AR.get(4 * 256, BF)]
        vst_free = [None, None]
        mark = AR.off
        cnt = [0]
        hn_prev = [None, None]
        srcs = [w_in[40 + i] for i in range(8)] + [w_in[48 + i] for i in range(8)]

        def body(j, sv):
            last = None
            if j < 8:
                AR.off = mark + (j % 2) * 2 * 2560
                ACT.wait(hn_prev[j % 2])
                DVE.wait(hn_prev[j % 2])
                for hh in range(2):
                    h = 2 * j + hh
                    b = get_bank()
                    e = mm_acc(b, banks[b][:, :], [(sv[:, kc, hh * 128:(hh + 1) * 128], xnT_t[:, kc, :]) for kc in range(32)])
                    last = e
                    k = cnt[0] % 2
                    cnt[0] += 1
                    DVE.wait(kst_free[k])
                    eo, esq, ekf = head_norm(banks[b][:, :], 512, hg_t[:, 2:3], kst[k], e, True)
                    bank_free[b] = ekf
                    SP.wait(eo)
                    kst_free[k] = dma(SP, sl_kst[k], KT[h][:, vt * 512:(vt + 1) * 512], kst[k])
                    hn_prev[j % 2] = eo
            else:
                s = j - 8
                k = s % 2
                ACT.wait(vst_free[k])
                ee = None
                for blk in range(4):
                    b = get_bank()
                    e = mm_acc(b, banks[b][:, 0:256], [(xnT_t[:, kc, blk * 128:(blk + 1) * 128], sv[:, kc, :]) for kc in range(32)])
                    last = e
                    ACT.wait(e)
                    ee = ACT.done(ACT.e.copy(out=vst[k][:, blk * 256:(blk + 1) * 256], in_=banks[b][:, 0:256]))
                    bank_free[b] = ee
                SP.wait(ee)
                dst = VS[vt * 512:(vt + 1) * 512, s * 256:(s + 1) * 256].rearrange("(b p) c -> p b c", p=128)
                vst_free[k] = dma(SP, sl_vst[k], dst, vst[k].rearrange("p (b c) -> p b c", c=256))
            return last
        with nc.named_scope("A_sbkv"):
            stream_slabs(srcs, [32] * 16, body)
            barrier()

        AR.reset()
        srcs = []
        for s in range(8):
            srcs += [w_in[8 + s], w_in[16 + s]]
        kd = AR.get(4 * 256, BF)
        kr16 = AR.get(4 * 256, BF)
        vb = AR.get(4 * 256, BF)
        cap = AR.get(256, BF)
        mark2 = AR.off
        hold = {}

        def body2(j, sv):
            s = j // 2
            last = None
            if j % 2 == 0:
                for blk in range(4):
                    AR.off = mark2
                    b = get_bank()
                    e = mm_acc(b, banks[b][:, 0:256], [(xnT_t[:, kc, blk * 128:(blk + 1) * 128], sv[:, kc, :]) for kc in range(32)])
                    last = e
                    krf = AR.get(256, F32)
                    e2, e1 = rope_tok(banks[b][:, 0:256], blk, krf, e)
                    bank_free[b] = e1
                    DVE.wait(e2, hold.get("kdma"))
                    kdb = rtab_t[:, 2 * s:2 * s + 2].unsqueeze(2).to_broadcast([128, 2, 128])
                    DVE.e.tensor_tensor(out=kd[:, blk * 256:(blk + 1) * 256].rearrange("p (h d) -> p h d", h=2),
                                        in0=krf.rearrange("p (h d) -> p h d", h=2), in1=kdb, op=ALU.mult)
                    ek = DVE.done(DVE.e.tensor_copy(out=kr16[:, blk * 256:(blk + 1) * 256], in_=krf))
                    hold[("k", blk)] = ek
                    DVE.wait(ek)
                if vt >= 11:
                    SP.wait(hold[("k", 3)])
                    r0 = vt * 512 - SCR0
                    dst = RKo[r0:r0 + 512, s * 256:(s + 1) * 256].rearrange("(b p) c -> p b c", p=128)
                    hold["kdma"] = dma(SP, sl_rk, dst, kr16.rearrange("p (b c) -> p b c", c=256))
            else:
                for blk in range(4):
                    b = get_bank()
                    e = mm_acc(b, banks[b][:, 0:256], [(xnT_t[:, kc, blk * 128:(blk + 1) * 128], sv[:, kc, :]) for kc in range(32)])
                    last = e
                    ACT.wait(e, hold.get("vdma"), hold.get("kvlast"))
                    ev = ACT.done(ACT.e.copy(out=vb[:, blk * 256:(blk + 1) * 256], in_=banks[b][:, 0:256]))
                    bank_free[b] = ev
                    hold[("v", blk)] = ev
                if vt >= 11:
                    SP.wait(hold[("v", 3)])
                    r0 = vt * 512 - SCR0
                    dst = RVo[r0:r0 + 512, s * 256:(s + 1) * 256].rearrange("(b p) c -> p b c", p=128)
                    hold["vdma"] = dma(SP, sl_rv, dst, vb.rearrange("p (b c) -> p b c", c=256))
                for blk in range(4):
                    n = vt * 4 + blk
                    b = get_bank()
                    PE.wait(bank_free[b], hold[("k", blk)], hold[("v", blk)])
                    ins = None
                    for hh in range(2):
                        ins = PE.e.matmul(banks[b][:, hh * 128:(hh + 1) * 128],
                                          kd[:, blk * 256 + hh * 128: blk * 256 + (hh + 1) * 128],
                                          vb[:, blk * 256 + hh * 128: blk * 256 + (hh + 1) * 128],
                                          start=(hh == 0), stop=(hh == 1), skip_group_check=True)
                    ekv = PE.done(ins)
                    last = ekv
                    st2 = state[:, 2 * s:2 * s + 2, :]
                    if n >= 47:
                        DVE.wait(hold.get("capdma"))
                        ec = DVE.done(DVE.e.tensor_copy(out=cap.rearrange("p (h d) -> p h d", h=2), in_=st2))
                        SP.wait(ec)
                        hold["capdma"] = dma(SP, sl_st, STs[n - 47][:, s * 256:(s + 1) * 256], cap)
                        DVE.wait(ec)
                    db = rtab_t[:, 32 + 2 * s:32 + 2 * s + 2].unsqueeze(2).to_broadcast([128, 2, 128])
                    ed = DVE.done(DVE.e.tensor_tensor(out=st2, in0=st2, in1=db, op=ALU.mult))
                    DVE.wait(ed, ekv)
                    eu = DVE.done(DVE.e.tensor_tensor(out=st2, in0=st2,
                                                      in1=banks[b][:, 0:256].rearrange("p (h d) -> p h d", h=2), op=ALU.add))
                    bank_free[b] = eu
                    DVE.wait(eu)
            return last
        with nc.named_scope("A_ret"):
            stream_slabs(srcs, [32] * 16, body2)
            xnT_free[0] = (PE.sem, PE.cnt)
            barrier()

    def barrier_light():
        es = [PE, ACT, DVE]
        evs = [(e.sem, e.cnt) for e in es if e.cnt > 0]
        for e in es:
            e.wait(*evs)

    def own_pipeline(v0, ntok, c0, out_row0):
        nblk = ntok // 128
        orow = v0 - HALO0
        chunk0 = v0 // 128 - 47
        xq = xnT_t[:, :, c0:c0 + ntok]

        AR.reset()
        ev_rope = load_rope(v0 // 128, nblk)
        DVE.wait(ev_rope)
        qtok = AR.get(4 * 256, BF)
        ktok = AR.get(4 * 256, BF)
        vtok = AR.get(4 * 256, BF)
        sttok = AR.get(4 * 256, BF)
        qT = AR.get(2 * 512, BF)
        kT = AR.get(2 * 512, BF)
        sg = AR.get(2 * 512, F32)
        Sp = AR.get(512, BF)
        of = AR.get(512, F32)
        mark = AR.off
        srcs = []
        for s in range(8):
            srcs += [w_in[0 + s], w_in[24 + s]]
        hold = {}

        def body(j, sv):
            s = j // 2
            last = None
            r0 = v0 - SCR0
            if j % 2 == 0:
                SP.wait(hold.get("ret_done"))
                dma(SP, sl_k[0], ktok[:, 0:nblk * 256].rearrange("p (b c) -> p b c", c=256),
                    RKo[r0:r0 + ntok, s * 256:(s + 1) * 256].rearrange("(b p) c -> p b c", p=128))
                dma(SP, sl_k[0], vtok[:, 0:nblk * 256].rearrange("p (b c) -> p b c", c=256),
                    RVo[r0:r0 + ntok, s * 256:(s + 1) * 256].rearrange("(b p) c -> p b c", p=128))
                dma(SP, sl_k[0], qrow_t[:], qrow[:, s * 256:(s + 1) * 256].rearrange("p (h c) -> p h c", h=2))
                hold["ld"] = dma(SP, sl_k[0], sttok[:, 0:nblk * 256].rearrange("p (b c) -> p b c", c=256),
                                 STs[chunk0:chunk0 + nblk, :, s * 256:(s + 1) * 256].rearrange("b p c -> p b c"))
                for blk in range(nblk):
                    AR.off = mark
                    b = get_bank()
                    e = mm_acc(b, banks[b][:, 0:256], [(xq[:, kc, blk * 128:(blk + 1) * 128], sv[:, kc, :]) for kc in range(32)])
                    last = e
                    qf = AR.get(256, F32)
                    e2, e1 = rope_tok(banks[b][:, 0:256], blk, qf, e)
                    bank_free[b] = e1
                    DVE.wait(e2, hold.get("ret_done"))
                    eq = DVE.done(DVE.e.tensor_copy(out=qtok[:, blk * 256:(blk + 1) * 256], in_=qf))
                    hold[("q", blk)] = eq
                    DVE.wait(eq)
                PE.wait(hold["ld"], hold[("q", nblk - 1)])
                for (src_t, dstT, nm) in ((qtok, qT, "qT"), (ktok, kT, "kT")):
                    bi = 6 if nm == "qT" else 7
                    pt = banks[bi].bitcast(BF).rearrange("p (a b) -> p a b", b=128)
                    PE.wait(bank_free[bi])
                    ins = None
                    for hh in range(2):
                        for blk in range(nblk):
                            ins = PE.e.transpose(pt[:, hh * 4 + blk, :],
                                                 src_t[:, blk * 256 + hh * 128: blk * 256 + (hh + 1) * 128], ident)
                    te = PE.done(ins)
                    last = te
                    ACT.wait(te, hold.get("ret_done"))
                    ee = None
                    for hh in range(2):
                        ee = ACT.e.copy(out=dstT[:, hh * 512: hh * 512 + ntok].rearrange("p (b c) -> p b c", c=128),
                                        in_=pt[:, hh * 4: hh * 4 + nblk, :])
                    ee = ACT.done(ee)
                    bank_free[bi] = ee
                    hold[nm] = ee
            else:
                for hh in range(2):
                    b = get_bank()
                    e = mm_acc(b, banks[b][:, 0:ntok], [(sv[:, kc, hh * 128:(hh + 1) * 128], xq[:, kc, :]) for kc in range(32)])
                    last = e
                    ACT.wait(e, hold.get("ret_done"))
                    eg = ACT.done(ACT.e.activation(out=sg[:, hh * 512: hh * 512 + ntok], in_=banks[b][:, 0:ntok], func=AF.Silu))
                    bank_free[b] = eg
                    hold[("sg", hh)] = eg
                for hh in range(2):
                    h = 2 * s + hh
                    AR.off = mark
                    b = get_bank()
                    PE.wait(bank_free[b], hold["qT"], hold["kT"])
                    ins = None
                    for blk in range(nblk):
                        ins = PE.e.matmul(banks[b][:, blk * 128:(blk + 1) * 128],
                                          kT[:, hh * 512 + blk * 128: hh * 512 + (blk + 1) * 128],
                                          qT[:, hh * 512 + blk * 128: hh * 512 + (blk + 1) * 128],
                                          start=(blk == 0), stop=(blk == nblk - 1), skip_group_check=True)
                    es = PE.done(ins)
                    DVE.wait(es, hold.get(("o", hh ^ 1)), hold.get(("o", hh)))
                    m3 = mask01_f[:, :].unsqueeze(1).to_broadcast([128, nblk, 128])
                    esp = DVE.done(DVE.e.scalar_tensor_tensor(
                        out=Sp[:, 0:ntok].rearrange("p (b c) -> p b c", c=128),
                        in0=banks[b][:, 0:ntok].rearrange("p (b c) -> p b c", c=128),
                        scalar=rtab_t[:, 16 + h:16 + h + 1], in1=m3, op0=ALU.mult, op1=ALU.mult))
                    bank_free[b] = esp
                    b2 = get_bank()
                    PE.wait(bank_free[b2], esp, hold["ld"])
                    ins = None
                    for blk in range(nblk):
                        PE.e.matmul(banks[b2][:, blk * 128:(blk + 1) * 128],
                                    vtok[:, blk * 256 + hh * 128: blk * 256 + (hh + 1) * 128],
                                    Sp[:, blk * 128:(blk + 1) * 128],
                                    start=(blk == 0), stop=False, skip_group_check=True)
                        ins = PE.e.matmul(banks[b2][:, blk * 128:(blk + 1) * 128],
                                          sttok[:, blk * 256 + hh * 128: blk * 256 + (hh + 1) * 128],
                                          qT[:, hh * 512 + blk * 128: hh * 512 + (blk + 1) * 128],
                                          start=False, stop=True, skip_group_check=True)
                    eo = PE.done(ins)
                    last = eo
                    DVE.wait(eo)
                    q3 = qrow_t[:, hh, :].unsqueeze(1).to_broadcast([128, nblk, 128])
                    eof = DVE.done(DVE.e.tensor_tensor(out=of[:, 0:ntok].rearrange("p (b c) -> p b c", c=128),
                                                       in0=banks[b2][:, 0:ntok].rearrange("p (b c) -> p b c", c=128),
                                                       in1=q3, op=ALU.mult))
                    bank_free[b2] = eof
                    on = AR.get(512, F32)
                    eon, esq, ekf = head_norm(of[:, 0:ntok], ntok, hg_t[:, 0:1], on[:, 0:ntok], eof, False)
                    DVE.wait(eon, hold[("sg", hh)])
                    em = DVE.done(DVE.e.tensor_tensor(out=mixT[:, h, 0:ntok], in0=on[:, 0:ntok],
                                                      in1=sg[:, hh * 512: hh * 512 + ntok], op=ALU.mult))
                    hold[("o", hh)] = em
                    DVE.wait(em, esq)
                    barrier_light()
                hold["ret_done"] = (DVE.sem, DVE.cnt)
            return last
        with nc.named_scope("B_ret"):
            stream_slabs(srcs, [32] * 16, body)
            barrier()
        checkpoint(f"ret@{v0}")

        AR.reset()
        qTs = AR.get(2 * 512, BF)
        NKB = 3
        kbuf = [AR.get(2 * 512, BF).rearrange("p (h c) -> p h c", h=2) for _ in range(NKB)]
        vbuf = [AR.get(2 * 512, BF).rearrange("p (h c) -> p h c", h=2) for _ in range(NKB)]
        ebuf = [AR.get(2 * 512, F32).rearrange("p (h c) -> p h c", h=2) for _ in range(2)]
        spb = [AR.get(2 * 512, F16).rearrange("p (h c) -> p h c", h=2) for _ in range(2)]
        gbuf = AR.get(2 * 512, F32).rearrange("p (h c) -> p h c", h=2)
        abuf = AR.get(2 * 512, BF).rearrange("p (h c) -> p h c", h=2)
        of2 = AR.get(2 * 512, F32).rearrange("p (h c) -> p h c", h=2)
        mark3 = AR.off
        srcs = [w_in[32 + s_] for s_ in range(8)]
        kb_max = (v0 + ntok) // 128 - 1
        qblk0 = v0 // 128
        ktiles = list(range(kb_max // 4, -1, -1))
        blocks = []
        for kt in ktiles:
            for kb in range(min(kb_max, kt * 4 + 3), kt * 4 - 1, -1):
                blocks.append((kt, kb))
        nb = len(blocks)
        Zp = [pairs[0][:, :].rearrange("p (h c) -> p h c", h=2), pairs[1][:, :].rearrange("p (h c) -> p h c", h=2)]
        Cp = pairs[2][:, :].rearrange("p (h c) -> p h c", h=2)
        Op = pairs[3][:, :].rearrange("p (h c) -> p h c", h=2)
        hold2 = {}
        kvslot_cnt = [0]

        def body3(j, sv):
            last = None
            qev = []
            for hh in range(2):
                AR.off = mark3
                b = get_bank((0, 1, 2, 3))
                e = mm_acc(b, banks[b][:, 0:ntok], [(sv[:, kc, hh * 128:(hh + 1) * 128], xq[:, kc, :]) for kc in range(32)])
                last = e
                DVE.wait(hold2.get("att_done"))
                eo, esq, ekf = head_norm(banks[b][:, 0:ntok], ntok, hg_t[:, 1:2], qTs[:, hh * 512: hh * 512 + ntok], e, True)
                bank_free[b] = ekf
                qev.append(eo)
                DVE.wait(esq)
            q3 = qTs.rearrange("p (h c) -> p h c", h=2)
            st = {"kvev": {}, "kvfree": [hold2.get("att_done")] * NKB, "Zfree": [None, None], "efree": [None, None], "spfree": [None, None],
                  "gfree": None, "afree": None, "firstC": [True, True], "firstO": [True, True]}
            evQK, evE, evL, evM1, evG, evA, evAV = {}, {}, {}, {}, {}, {}, {}

            def geom(i):
                kt, kb = blocks[i]
                r = kb - qblk0
                col0 = max(r, 0) * 128
                return kt, kb, kb - kt * 4, r, col0

            def load_kv(kt):
                if kt in st["kvev"] or kt < 0:
                    return
                k = kt % NKB
                SP.wait(st["kvfree"][k])
                ev = None
                for hh in range(2):
                    h = 2 * j + hh
                    dma(SP, sl_kv[k], kbuf[k][:, hh, :], KT[h][:, kt * 512:(kt + 1) * 512])
                    ev = dma(SP, sl_kv[k], vbuf[k][:, hh, :].rearrange("p (b c) -> p b c", c=128),
                             VS[kt * 512:(kt + 1) * 512, h * 128:(h + 1) * 128].rearrange("(b p) c -> p b c", p=128))
                st["kvev"][kt] = ev

            def QK(i):
                kt, kb, kl, r, col0 = geom(i)
                load_kv(kt)
                load_kv(kt - 1)
                z = Zp[i % 2]
                PE.wait(st["Zfree"][i % 2], st["kvev"][kt], *qev)
                if i < 2:
                    PE.wait(bank_free[0], bank_free[1], bank_free[2], bank_free[3])
                ins = None
                for hh in range(2):
                    ins = PE.e.matmul(z[:, hh, col0:ntok], kbuf[kt % NKB][:, hh, kl * 128:(kl + 1) * 128], q3[:, hh, col0:ntok],
                                      start=True, stop=True)
                evQK[i] = PE.done(ins)

            def EXP(i):
                kt, kb, kl, r, col0 = geom(i)
                ACT.wait(evQK[i], st["efree"][i % 2])
                evE[i] = ACT.done(ACT.e.activation(out=ebuf[i % 2][:, :, col0:ntok], in_=Zp[i % 2][:, :, col0:ntok], func=AF.Exp, scale=SCALE))
                st["Zfree"][i % 2] = evE[i]

            def LN(i):
                kt, kb, kl, r, col0 = geom(i)
                ACT.wait(evE[i], st["spfree"][i % 2])
                ev = ACT.done(ACT.e.activation(out=spb[i % 2][:, :, col0:ntok], in_=ebuf[i % 2][:, :, col0:ntok], func=AF.Ln, bias=cst_t[:, 1:2]))
                if r >= 0:
                    DVE.wait(ev)
                    mb = dmask_h[:, :].unsqueeze(1).to_broadcast([128, 2, 128])
                    ev = DVE.done(DVE.e.tensor_tensor(out=spb[i % 2][:, :, col0:col0 + 128], in0=spb[i % 2][:, :, col0:col0 + 128],
                                                      in1=mb, op=ALU.mult))
                evL[i] = ev

            def MM1(i):
                kt, kb, kl, r, col0 = geom(i)
                PE.wait(evL[i])
                if i == 0:
                    PE.wait(bank_free[4], bank_free[5], bank_free[6], bank_free[7])
                ins = None
                for hh in range(2):
                    ins = PE.e.matmul(Cp[:, hh, col0:ntok], Umat, spb[i % 2][:, hh, col0:ntok], start=st["firstC"][hh], stop=False,
                                      skip_group_check=True)
                    st["firstC"][hh] = False
                evM1[i] = PE.done(ins)

            def G(i):
                kt, kb, kl, r, col0 = geom(i)
                ACT.wait(evM1[i], st["gfree"])
                evG[i] = ACT.done(ACT.e.activation(out=gbuf[:, :, col0:ntok], in_=Cp[:, :, col0:ntok], func=AF.Exp, scale=-1.0))

            def MM2(i):
                kt, kb, kl, r, col0 = geom(i)
                PE.wait(evG[i])
                ins = None
                for hh in range(2):
                    ins = PE.e.matmul(Cp[:, hh, col0:ntok], Lmat, spb[i % 2][:, hh, col0:ntok], start=False, stop=False,
                                      skip_group_check=True)
                st["spfree"][i % 2] = PE.done(ins)

            def MUL(i):
                kt, kb, kl, r, col0 = geom(i)
                DVE.wait(evG[i], st["afree"])
                ea = DVE.done(DVE.e.tensor_tensor(out=abuf[:, :, col0:ntok], in0=ebuf[i % 2][:, :, col0:ntok], in1=gbuf[:, :, col0:ntok], op=ALU.mult))
                st["gfree"] = ea
                st["efree"][i % 2] = ea
                if r >= 0:
                    DVE.wait(ea)
                    mb = cm_t[:, 384:512].unsqueeze(1).to_broadcast([128, 2, 128])
                    ea = DVE.done(DVE.e.tensor_tensor(out=abuf[:, :, col0:col0 + 128], in0=abuf[:, :, col0:col0 + 128], in1=mb, op=ALU.mult))
                evA[i] = ea

            def AV(i):
                kt, kb, kl, r, col0 = geom(i)
                PE.wait(evA[i])
                ins = None
                for hh in range(2):
                    ins = PE.e.matmul(Op[:, hh, col0:ntok], vbuf[kt % NKB][:, hh, kl * 128:(kl + 1) * 128], abuf[:, hh, col0:ntok],
                                      start=st["firstO"][hh], stop=False, skip_group_check=True)
                    st["firstO"][hh] = False
                evAV[i] = PE.done(ins)
                st["afree"] = evAV[i]
                if i + 1 == nb or blocks[i + 1][0] != kt:
                    st["kvfree"][kt % NKB] = evAV[i]
                    st["kvev"].pop(kt + NKB, None)

            QK(0)
            EXP(0)
            LN(0)
            if nb > 1:
                QK(1)
            for i in range(nb):
                MM1(i)
                if i + 2 < nb:
                    QK(i + 2)
                if i + 1 < nb:
                    EXP(i + 1)
                G(i)
                if i + 1 < nb:
                    LN(i + 1)
                MM2(i)
                MUL(i)
                AV(i)
            last = evAV[nb - 1]
            DVE.wait(last)
            eof = DVE.done(DVE.e.tensor_copy(out=of2[:, :, 0:ntok], in_=Op[:, :, 0:ntok]))
            for bi in range(8):
                bank_free[bi] = eof
            for hh in range(2):
                h = 2 * j + hh
                AR.off = mark3
                eon, esq, ekf = head_norm(of2[:, hh, 0:ntok], ntok, hg_t[:, 3:4], mixT[:, 16 + h, 0:ntok], eof, False)
                DVE.wait(eon, esq)
            barrier_light()
            hold2["att_done"] = (DVE.sem, DVE.cnt)
            return last
        with nc.named_scope("B_sb"):
            stream_slabs(srcs, [32] * 8, body3)
            barrier()
        checkpoint(f"sb@{v0}")

        def proj_residual(wsrc, nslab, kcs, lhs_of, res_rows, dst_rows, final=False):
            rbuf = [AR.get(4 * 256, F32), AR.get(4 * 256, F32)]
            rfree = [None, None]
            cnt2 = [0]

            def bodyp(j, sv):
                k = cnt2[0] % 2
                cnt2[0] += 1
                SP.wait(rfree[k])
                eld = dma(SP, sl_rld[k], rbuf[k][:, 0:nblk * 256].rearrange("p (b c) -> p b c", c=256),
                          res_rows[:, j * 256:(j + 1) * 256].rearrange("(b p) c -> p b c", p=128))
                last = None
                ee = None
                for blk in range(nblk):
                    b = get_bank()
                    e = mm_acc(b, banks[b][:, 0:256], [(lhs_of(kc, blk), sv[:, kc, :]) for kc in range(kcs)])
                    last = e
                    DVE.wait(e, eld)
                    ee = DVE.done(DVE.e.tensor_tensor(out=rbuf[k][:, blk * 256:(blk + 1) * 256], in0=rbuf[k][:, blk * 256:(blk + 1) * 256],
                                                      in1=banks[b][:, 0:256], op=ALU.add))
                    bank_free[b] = ee
                SP.wait(ee)
                rfree[k] = dma(SP, sl_rst[k], dst_rows[:, j * 256:(j + 1) * 256].rearrange("(b p) c -> p b c", p=128),
                               rbuf[k][:, 0:nblk * 256].rearrange("p (b c) -> p b c", c=256))
                return last
            stream_slabs([wsrc[i] for i in range(nslab)], [kcs] * nslab, bodyp)
            barrier()

        xrows = x[v0:v0 + ntok, :]
        AR.reset()
        with nc.named_scope("B_wout"):
            proj_residual(w_out, 16, 32, lambda kc, blk: mixT[:, kc, blk * 128:(blk + 1) * 128], xrows, H1[orow:orow + ntok, :])
        checkpoint(f"h1@{v0}")

        norm_T(lambda blk: H1[orow + blk * 128: orow + (blk + 1) * 128, :], nblk, 1)
        AR.reset()
        qc = AR.get(4 * 512, BF)
        oc = AR.get(4 * 512, BF)
        pbuf = AR.get(256, F32)
        pb16 = AR.get(256, BF)
        pT = AR.get(256, BF)
        mark4 = AR.off

        def bodyq(j, sv):
            last = None
            for hh in range(2):
                h = 2 * j + hh
                AR.off = mark4
                b = get_bank()
                e = mm_acc(b, banks[b][:, 0:ntok], [(sv[:, kc, hh * 128:(hh + 1) * 128], xnT_t[:, kc, 0:ntok]) for kc in range(32)])
                last = e
                eo, esq, ekf = head_norm(banks[b][:, 0:ntok], ntok, hg_t[:, 4:5], qc[:, h * 512: h * 512 + ntok], e, True)
                bank_free[b] = ekf
                DVE.wait(eo, esq)
                barrier_light()
            return last
        stream_slabs([w_cq[0], w_cq[1]], [32, 32], bodyq)
        xnT_free[0] = (PE.sem, PE.cnt)
        barrier()
        for h in range(4):
            for blk in range(nblk):
                b = get_bank()
                e = mm_acc(b, banks[b][:, 0:256], [(qc[:, h * 512 + blk * 128: h * 512 + (blk + 1) * 128], kmT[:, h, :])])
                DVE.wait(e)
                e0 = DVE.done(DVE.e.tensor_reduce(out=ss[:, 4:5], in_=banks[b][:, 0:256], axis=AX.X, op=ALU.max))
                DVE.wait(e0)
                e1 = DVE.done(DVE.e.tensor_scalar(out=ss[:, 5:6], in0=ss[:, 4:5], scalar1=-SCALE, scalar2=None, op0=ALU.mult))
                ACT.wait(e1)
                e2 = ACT.done(ACT.e.activation(out=pbuf, in_=banks[b][:, 0:256], func=AF.Exp, bias=ss[:, 5:6], scale=SCALE,
                                               accum_out=ss[:, 6:7]))
                bank_free[b] = e2
                DVE.wait(e2)
                e3 = DVE.done(DVE.e.reciprocal(out=ss[:, 7:8], in_=ss[:, 6:7]))
                DVE.wait(e3)
                e4 = DVE.done(DVE.e.tensor_scalar(out=pb16, in0=pbuf, scalar1=ss[:, 7:8], scalar2=None, op0=ALU.mult))
                pt = banks[6].bitcast(BF).rearrange("p (a b) -> p a b", b=128)
                PE.wait(e4, bank_free[6])
                PE.e.transpose(pt[:, 0, :], pb16[:, 0:128], ident)
                te = PE.done(PE.e.transpose(pt[:, 1, :], pb16[:, 128:256], ident))
                ACT.wait(te)
                e5 = ACT.done(ACT.e.copy(out=pT.rearrange("p (a b) -> p a b", b=128), in_=pt[:, 0:2, :]))
                bank_free[6] = e5
                b2 = get_bank()
                e6 = mm_acc(b2, banks[b2][:, 0:128], [(vm[:, mb, h * 128:(h + 1) * 128], pT[:, mb * 128:(mb + 1) * 128]) for mb in range(2)],
                            extra_wait=(e5,))
                ACT.wait(e6)
                e7 = ACT.done(ACT.e.copy(out=oc[:, h * 512 + blk * 128: h * 512 + (blk + 1) * 128], in_=banks[b2][:, 0:128]))
                bank_free[b2] = e7
                barrier_light()
        barrier()
        proj_residual(w_co, 16, 4, lambda kc, blk: oc[:, kc * 512 + blk * 128: kc * 512 + (blk + 1) * 128],
                      H1[orow:orow + ntok, :], H2[orow:orow + ntok, :])

        checkpoint(f"h2@{v0}")
        norm_T(lambda blk: H2[orow + blk * 128: orow + (blk + 1) * 128, :], nblk, 2)
        is_halo = (ntok == 128)
        AR.reset()
        ARF = [sb_ff[0], sb_ff[1]]

        def bodyu(j, sv):
            last = None
            cs = []
            for gv in range(2):
                ub = ARF[gv]
                b = get_bank()
                e = mm_acc(b, banks[b][:, 0:ntok], [(sv[:, kc, gv * 128:(gv + 1) * 128], xnT_t[:, kc, 0:ntok]) for kc in range(32)])
                last = e
                ACT.wait(e, fhold.get(("c", gv)))
                ACT.e.copy(out=ub[:, 0:2], in_=carry[:, j * 2 + gv, :])
                eu = ACT.done(ACT.e.copy(out=ub[:, 2:2 + ntok], in_=banks[b][:, 0:ntok]))
                bank_free[b] = eu
                DVE.wait(eu, fhold.get("mul"))
                base = (j * 2 + gv) * 4
                cb = cbuf[gv]
                if not is_halo:
                    ea = DVE.done(DVE.e.tensor_scalar(out=cb[:, 0:ntok], in0=ub[:, 2:2 + ntok], scalar1=cw_t[:, base + 2:base + 3],
                                        scalar2=cw_t[:, base + 3:base + 4], op0=ALU.mult, op1=ALU.add))
                    DVE.wait(ea)
                    ea = DVE.done(DVE.e.scalar_tensor_tensor(out=cb[:, 0:ntok], in0=ub[:, 1:1 + ntok], scalar=cw_t[:, base + 1:base + 2],
                                               in1=cb[:, 0:ntok], op0=ALU.mult, op1=ALU.add))
                    DVE.wait(ea)
                    ec = DVE.done(DVE.e.scalar_tensor_tensor(out=cb[:, 0:ntok], in0=ub[:, 0:ntok], scalar=cw_t[:, base:base + 1],
                                                             in1=cb[:, 0:ntok], op0=ALU.mult, op1=ALU.add))
                    cs.append(ec)
                ek = DVE.done(DVE.e.tensor_copy(out=carry[:, j * 2 + gv, :], in_=ub[:, ntok:ntok + 2]))
                fhold[("c", gv)] = ek
            if not is_halo:
                ACT.wait(cs[0])
                es = ACT.done(ACT.e.activation(out=cbuf[0][:, 0:ntok], in_=cbuf[0][:, 0:ntok], func=AF.Silu))
                DVE.wait(es, cs[1])
                em = DVE.done(DVE.e.tensor_tensor(out=aT[:, j, 0:ntok], in0=cbuf[0][:, 0:ntok], in1=cbuf[1][:, 0:ntok], op=ALU.mult))
                fhold["mul"] = em
            return last
        fhold = {}
        with nc.named_scope("B_up"):
            stream_slabs([w_up[i] for i in range(NFC)], [32] * NFC, bodyu)
        xnT_free[0] = (PE.sem, PE.cnt)
        barrier()
        checkpoint(f"up@{v0}")
        if is_halo:
            DVE.done(DVE.e.tensor_scalar(out=carry[:], in0=carry[:], scalar1=flag_t[:, 0:1], scalar2=None, op0=ALU.mult))
            barrier()
            return
        srcs = []
        kcs = []
        for cg in range(16):
            srcs += [w_dn0[cg], w_dn1[cg], w_dn2[cg]]
            kcs += [32, 32, 22]
        dbank = [[0, 1, 2, 3], [4, 5, 0, 1]]
        rb = [fbuf[0], fbuf[1]]
        rfree = [None, None]
        dst_rows = y[out_row0:out_row0 + ntok, :]
        res_rows = H2[orow:orow + ntok, :]
        dstate = {}

        def bodyd(j, sv):
            cg, kg = j // 3, j % 3
            k = cg % 2
            bs = [0, 1, 2, 3] if k == 0 else [4, 5, 6, 7]
            if kg == 0:
                SP.wait(rfree[k])
                dstate["ld"] = dma(SP, sl_rld[k], rb[k].rearrange("p (b c) -> p b c", c=256),
                                   res_rows[:, cg * 256:(cg + 1) * 256].rearrange("(b p) c -> p b c", p=128))
            last = None
            koff = kg * 32
            nk = kcs[j]
            for blk in range(4):
                b = bs[blk]
                if kg == 0:
                    PE.wait(bank_free[b])
                ins = None
                for kc in range(nk):
                    ins = PE.e.matmul(banks[b][:, 0:256], aT[:, koff + kc, blk * 128:(blk + 1) * 128], sv[:, kc, :],
                                      start=(kg == 0 and kc == 0), stop=(kg == 2 and kc == nk - 1))
                e = PE.done(ins)
                last = e
                if kg == 2:
                    DVE.wait(e, dstate["ld"])
                    ee = DVE.done(DVE.e.tensor_tensor(out=rb[k][:, blk * 256:(blk + 1) * 256], in0=rb[k][:, blk * 256:(blk + 1) * 256],
                                                      in1=banks[b][:, 0:256], op=ALU.add))
                    bank_free[b] = ee
                    dstate["ee"] = ee
            if kg == 2:
                SP.wait(dstate["ee"])
                rfree[k] = dma(SP, sl_rst[k], dst_rows[:, cg * 256:(cg + 1) * 256].rearrange("(b p) c -> p b c", p=128),
                               rb[k].rearrange("p (b c) -> p b c", c=256))
            return last
        with nc.named_scope("B_down"):
            stream_slabs(srcs, kcs, bodyd)
            barrier()

    sb_ff = [xs[:, 0:516], xs[:, 520:1036]]
    cbuf = [xs[:, 1040:1552], xs[:, 1552:2064]]
    fbuf = [xs[:, 2064:3088], xb[:, 0:2048].bitcast(F32)]

    try:
        checkpoint("const")
        mem_kv()
        checkpoint("mem")
        for vt in range(16):
            with nc.named_scope("A_norm"):
                norm_T(lambda blk, vt=vt: x[vt * 512 + blk * 128: vt * 512 + (blk + 1) * 128, :], 4, 0)
            checkpoint(f"norm{vt}")
            kv_side(vt)
            checkpoint(f"kv{vt}")
            if vt == 11:
                own_pipeline(HALO0, 128, 384, 0)
                checkpoint("halo")
            if vt >= 12:
                own_pipeline(vt * 512, 512, 0, (vt - 12) * 512)
                checkpoint(f"own{vt}")
    except _Stop:
        pass
    barrier()
    if debug:
        dbg = nc.dram_tensor("dbg_xnT", [128, 32 * 512], BF, kind="ExternalOutput").ap()
        ev = dma(SP, sl_dbg, dbg, xnT_t[:].rearrange("p c t -> p (c t)"))
        dbg2 = nc.dram_tensor("dbg_km", [128, 4 * 256 + 2 * 512], BF, kind="ExternalOutput").ap()
        dma(SP, sl_dbg, dbg2[:, 0:1024], kmT[:].rearrange("p c t -> p (c t)"))
        ev = dma(SP, sl_dbg, dbg2[:, 1024:2048], vm[:].rearrange("p c t -> p (c t)"))
        SP.wait(ev)
    nc._slab_trace = rec_trace
    return nc


def _pack(W):
    K, N = W.shape
    KC, NS = K // 128, N // 256
    return np.ascontiguousarray(W.reshape(KC, 128, NS, 256).transpose(2, 1, 0, 3)).reshape(NS, 128, KC * 256)


_CACHE = {}


def kernel(x, mem, attn_norm, w_in, ret_norm, sb_q_norm, sb_k_norm, sb_out_norm, w_out,
           cross_norm, mem_norm, cross_w_q, cross_w_kv, cross_q_norm, cross_k_norm, cross_w_o,
           ffn_norm, ffn_w_up, ffn_conv_w, ffn_conv_b, ffn_w_down):
    f32 = np.float32
    x = np.asarray(x, f32)
    mem = np.asarray(mem, f32)
    common = {}
    common["w_in"] = _pack(np.asarray(w_in[0], f32))
    common["w_out"] = _pack(np.asarray(w_out[0], f32))
    common["w_cq"] = _pack(np.asarray(cross_w_q[0], f32))
    common["w_ckv"] = _pack(np.asarray(cross_w_kv[0], f32))
    common["w_co"] = _pack(np.asarray(cross_w_o[0], f32))
    wu = np.asarray(ffn_w_up[0], f32)
    wup = np.empty((D, NFC, 2, 128), f32)
    wup[:, :, 0, :] = wu[:, :DFF].reshape(D, NFC, 128)
    wup[:, :, 1, :] = wu[:, DFF:].reshape(D, NFC, 128)
    common["w_up"] = _pack(wup.reshape(D, NFC * 256))
    del wup
    wd = np.asarray(ffn_w_down[0], f32)
    common["w_dn0"] = _pack(wd[0:4096])
    common["w_dn1"] = _pack(wd[4096:8192])
    common["w_dn2"] = _pack(wd[8192:DFF])

    def fcol(g):
        return np.asarray(g, f32).reshape(32, 128).T
    common["gcols"] = np.ascontiguousarray(np.concatenate(
        [fcol(attn_norm[0]), fcol(cross_norm[0]), fcol(ffn_norm[0]), fcol(mem_norm[0])], axis=1))
    hgm = np.zeros((128, 8), f32)
    for i, g in enumerate((ret_norm, sb_q_norm, sb_k_norm, sb_out_norm, cross_q_norm, cross_k_norm)):
        hgm[:, i] = np.asarray(g[0], f32)
    common["hg"] = hgm
    cwv = np.asarray(ffn_conv_w[0], f32)
    cbv = np.asarray(ffn_conv_b[0], f32)
    cwt = np.zeros((128, NFC, 2, 4), f32)
    for gv in range(2):
        for t in range(3):
            cwt[:, :, gv, t] = cwv[t, gv * DFF:(gv + 1) * DFF].reshape(NFC, 128).T
        cwt[:, :, gv, 3] = cbv[gv * DFF:(gv + 1) * DFF].reshape(NFC, 128).T
    common["cw"] = cwt.reshape(128, NFC * 8)
    hh = np.arange(NH, dtype=np.float64)
    log_g = np.log1p(-np.exp2(-5.0 - hh))
    p = np.arange(128, dtype=np.float64)
    rt = np.zeros((128, 48), f32)
    rt[:, 0:16] = np.exp((127 - p)[:, None] * log_g[None, :])
    rt[:, 16:32] = np.exp(-(p + 1)[:, None] * log_g[None, :])
    rt[:, 32:48] = np.exp(128 * log_g)[None, :]
    common["rtab"] = rt
    qr = (np.exp((p + 1)[None, :] * log_g[:, None]) * SCALE).astype(f32)
    common["qrow"] = np.ascontiguousarray(np.broadcast_to(qr.reshape(1, NH * 128), (128, NH * 128)))
    cm = np.zeros((128, 640), f32)
    cm[:, 0:128] = np.eye(128)
    cm[:, 128:256] = 1.0 / 128.0
    pi = np.arange(128)
    cm[:, 256:384] = (pi[:, None] <= pi[None, :])
    cm[:, 384:512] = (pi[:, None] < pi[None, :])
    common["cmats"] = cm.astype(ml_dtypes.bfloat16)
    hm = np.zeros((128, 256), np.float16)
    hm[:, 0:128] = (pi[:, None] >= pi[None, :])
    hm[:, 128:256] = (pi[:, None] < pi[None, :])
    common["hmats"] = hm

    inv_freq = (10000.0 ** (-np.linspace(0.0, 1.0, 64, dtype=f32))).astype(f32)
    in_maps = []
    for c in range(8):
        b, q = c // 4, c % 4
        shift = (3 - q) * 2048
        xp = np.zeros((S, D), f32)
        xp[shift:] = x[b, :S - shift]
        pos = (np.arange(S) - shift).astype(f32)
        pos = np.maximum(pos, 0).astype(f32)
        ang = (pos[:, None] * inv_freq[None, :]).astype(f32)
        co = np.cos(ang.astype(np.float64)).astype(f32).reshape(64, 128, 64).transpose(1, 0, 2).reshape(128, 4096)
        si = np.sin(ang.astype(np.float64)).astype(f32).reshape(64, 128, 64).transpose(1, 0, 2).reshape(128, 4096)
        m = dict(common)
        m["x"] = xp
        m["mem"] = np.ascontiguousarray(mem[b])
        m["ropec"] = np.ascontiguousarray(co)
        m["ropes"] = np.ascontiguousarray(si)
        m["ropen"] = np.ascontiguousarray(-si)
        m["flag"] = np.full((128, 1), 0.0 if q == 0 else 1.0, f32)
        in_maps.append(m)
    if _CACHE.get("prep_only"):
        return in_maps
    tr = build_program()._slab_trace
    nc = build_program(trace=tr)
    res = run_bass_kernel_spmd(nc, in_maps, core_ids=list(range(8)))
    out = np.zeros((2, S, D), f32)
    for c in range(8):
        b, q = c // 4, c % 4
        out[b, q * 2048:(q + 1) * 2048] = np.asarray(res.results[c]["y"], f32)
    return out
```

```python
import numpy as np
import ml_dtypes
import concourse.bass as bass
import concourse.mybir as mybir
from concourse.bass_utils import run_bass_kernel_spmd

AF = mybir.ActivationFunctionType
ALU = mybir.AluOpType
AX = mybir.AxisListType
F32, BF, F16 = mybir.dt.float32, mybir.dt.bfloat16, mybir.dt.float16

D = 4096
S = 8192
NH = 16
DFF = 11008
NFC = 86
EPS = 1e-6
OWN0 = 6144
HALO0 = 6016
OWNROWS = 2176
SCR0 = 5632
SCALE = 128 ** -0.5


class Eng:
    def __init__(self, nc, e, name):
        self.e = e
        self.sem = nc.alloc_semaphore("sem_" + name)
        self.cnt = 0
        self.seen = {}
        self.name = name

    def wait(self, *evs):
        for ev in evs:
            if ev is None:
                continue
            sem, val = ev
            k = id(sem)
            if self.seen.get(k, -1) >= val:
                continue
            self.seen[k] = val
            self.e.wait_ge(sem, val)

    def done(self, ins):
        self.cnt += 1
        ins.then_inc(self.sem, 1)
        return (self.sem, self.cnt)


class Slot:
    def __init__(self, nc, name):
        self.sem = nc.alloc_semaphore("dsem_" + name)
        self.cnt = 0

    def inc(self, ins, n=1):
        self.cnt += 16
        ins.then_inc(self.sem, 16)
        return (self.sem, self.cnt)


class _Stop(Exception):
    pass


def build_program(stop_after=None, debug=False, trace=None):
    nc = bass.Bass("TRN2", target_bir_lowering=False)
    rec_trace = []
    skind = "ExternalOutput" if debug else "Internal"

    def checkpoint(name):
        if stop_after is not None and name == stop_after:
            raise _Stop()

    def din(name, shape, dt=F32):
        return nc.dram_tensor(name, list(shape), dt, kind="ExternalInput").ap()

    x = din("x", [S, D])
    mem = din("mem", [256, D])
    w_in = din("w_in", [56, 128, 8192])
    w_out = din("w_out", [16, 128, 8192])
    w_cq = din("w_cq", [2, 128, 8192])
    w_ckv = din("w_ckv", [4, 128, 8192])
    w_co = din("w_co", [16, 128, 1024])
    w_up = din("w_up", [NFC, 128, 8192])
    w_dn0 = din("w_dn0", [16, 128, 8192])
    w_dn1 = din("w_dn1", [16, 128, 8192])
    w_dn2 = din("w_dn2", [16, 128, 22 * 256])
    gcols = din("gcols", [128, 4 * 32])
    hg = din("hg", [128, 8])
    cw = din("cw", [128, NFC * 2 * 4])
    rtab = din("rtab", [128, 3 * NH])
    qrow = din("qrow", [128, NH * 128])
    cmats = din("cmats", [128, 5 * 128], BF)
    hmats = din("hmats", [128, 2 * 128], F16)
    ropec = din("ropec", [128, 64 * 64])
    ropes = din("ropes", [128, 64 * 64])
    ropen = din("ropen", [128, 64 * 64])
    flag = din("flag", [128, 1])
    y = nc.dram_tensor("y", [2048, D], F32, kind="ExternalOutput").ap()

    KT = nc.dram_tensor("KT", [NH, 128, S], BF, kind=skind).ap()
    VS = nc.dram_tensor("VS", [S, 2048], BF, kind=skind).ap()
    RKo = nc.dram_tensor("RKo", [S - SCR0, 2048], BF, kind=skind).ap()
    RVo = nc.dram_tensor("RVo", [S - SCR0, 2048], BF, kind=skind).ap()
    STs = nc.dram_tensor("STs", [17, 128, 2048], BF, kind=skind).ap()
    H1 = nc.dram_tensor("H1", [OWNROWS, D], F32, kind=skind).ap()
    H2 = nc.dram_tensor("H2", [OWNROWS, D], F32, kind=skind).ap()

    PE = Eng(nc, nc.tensor, "pe")
    ACT = Eng(nc, nc.scalar, "act")
    DVE = Eng(nc, nc.vector, "dve")
    POOL = Eng(nc, nc.gpsimd, "pool")
    SP = Eng(nc, nc.sync, "sp")
    ENGS = [PE, ACT, DVE, POOL, SP]
    pending_dma = []

    def dma(q, slot, out, in_):
        ev = slot.inc(q.e.dma_start(out=out, in_=in_))
        pending_dma.append(ev)
        return ev

    def barrier():
        mx = {}
        for (sm, v) in pending_dma:
            if id(sm) not in mx or mx[id(sm)][1] < v:
                mx[id(sm)] = (sm, v)
        evs = [(e.sem, e.cnt) for e in ENGS if e.cnt > 0] + list(mx.values())
        for e in ENGS:
            e.wait(*evs)
        pending_dma.clear()

    def sb(name, shape, dt):
        return nc.alloc_sbuf_tensor("sb_" + name, list(shape), dt)

    slabs = [sb(f"slab{i}", [128, 8192], BF) for i in range(2)]
    slab_slot = [Slot(nc, f"slab{i}") for i in range(2)]
    slab_free = [None, None]
    xnT_t = sb("xnT", [128, 32, 512], BF)
    xs = sb("xs", [128, 4096], F32)
    xb = sb("xb", [128, 4096], BF)
    junk = xb
    big = sb("big", [128, NFC * 512], BF)
    gcols_t = sb("gcols", [128, 128], F32)
    hg_t = sb("hg", [128, 8], F32)
    cw_t = sb("cw", [128, NFC * 8], F32)
    rtab_t = sb("rtab", [128, 48], F32)
    cm_t = sb("cm", [128, 640], BF)
    hm_t = sb("hm", [128, 256], F16)
    dmask_f = sb("dmaskf", [128, 128], F32)
    dmask_h = sb("dmaskh", [128, 128], F16)
    mask01_f = sb("mask01f", [128, 128], F32)
    carry = sb("carry", [128, NFC * 2, 2], F32)
    state = sb("state", [128, NH, 128], F32)
    ss = sb("ss", [128, 8], F32)
    kmT = sb("kmT", [128, 4, 256], BF)
    vm = sb("vm", [128, 2, 512], BF)
    rp_c = sb("rp_c", [128, 4, 64], F32)
    rp_s = sb("rp_s", [128, 4, 64], F32)
    rp_n = sb("rp_n", [128, 4, 64], F32)
    qrow_t = sb("qrow_t", [128, 2, 128], F32)
    flag_t = sb("flag_t", [128, 1], F32)
    cst_t = sb("cst_t", [128, 4], F32)

    ident = cm_t[:, 0:128]
    ones = cm_t[:, 128:256]
    Umat = hm_t[:, 0:128]
    Lmat = hm_t[:, 128:256]

    mixT = big[:, 0:32 * 512].rearrange("p (c t) -> p c t", t=512)
    aT = big[:, :].rearrange("p (c t) -> p c t", t=512)
    SCR_OFF = 32 * 512

    class Arena:
        def __init__(self):
            self.off = SCR_OFF

        def reset(self):
            self.off = SCR_OFF

        def get(self, n, dt):
            mult = 2 if dt == F32 else 1
            self.off = (self.off + 15) // 16 * 16
            a = big[:, self.off:self.off + n * mult]
            self.off += n * mult
            assert self.off <= NFC * 512, "arena overflow"
            if dt == F32:
                return a.bitcast(F32)
            if dt == F16:
                return a.bitcast(F16)
            return a
    AR = Arena()

    pairs = [nc.alloc_psum_tensor(f"pp{i}", [128, 1024], F32) for i in range(4)]
    banks = [pairs[i // 2][:, (i % 2) * 512:(i % 2 + 1) * 512] for i in range(8)]
    bank_free = [None] * 8
    bank_rr = [0]

    def get_bank(choices=(0, 1, 2, 3, 4, 5)):
        i = choices[bank_rr[0] % len(choices)]
        bank_rr[0] += 1
        return i

    sl_const = Slot(nc, "const")
    sl_xs = Slot(nc, "xs")
    sl_xs2 = [Slot(nc, "xsa"), Slot(nc, "xsb")]
    sl_misc = Slot(nc, "misc")
    sl_out = Slot(nc, "out")
    sl_k = [Slot(nc, "k0"), Slot(nc, "k1")]
    sl_kv = [Slot(nc, "kv0"), Slot(nc, "kv1"), Slot(nc, "kv2")]
    sl_st = Slot(nc, "st")
    sl_kst = [Slot(nc, "kst0"), Slot(nc, "kst1")]
    sl_vst = [Slot(nc, "vst0"), Slot(nc, "vst1")]
    sl_rk = Slot(nc, "rk")
    sl_rv = Slot(nc, "rv")
    sl_rope = Slot(nc, "rope")
    sl_rld = [Slot(nc, "rld0"), Slot(nc, "rld1")]
    sl_rst = [Slot(nc, "rst0"), Slot(nc, "rst1")]
    sl_dbg = Slot(nc, "dbg")

    cev = []
    for dst, src in ((gcols_t, gcols), (hg_t, hg), (cw_t, cw), (rtab_t, rtab), (cm_t, cmats), (hm_t, hmats), (flag_t, flag)):
        cev.append(dma(SP, sl_const, dst[:], src))
    for e in (ACT, DVE, PE, POOL):
        e.wait(cev[-1])
    DVE.e.tensor_copy(out=dmask_f[:], in_=cm_t[:, 384:512])
    DVE.e.tensor_copy(out=dmask_h[:], in_=cm_t[:, 384:512])
    DVE.e.tensor_copy(out=mask01_f[:], in_=cm_t[:, 256:384])
    DVE.e.memset(carry[:], 0.0)
    DVE.e.memset(cst_t[:, 0:1], EPS)
    DVE.e.memset(cst_t[:, 1:2], 1.0)
    DVE.done(DVE.e.memset(state[:], 0.0))
    barrier()

    slab_idx = [0]

    WB = {}
    for nm, ns, ncl in (("w_in", 56, 8192), ("w_out", 16, 8192), ("w_cq", 2, 8192), ("w_co", 16, 1024),
                        ("w_up", NFC, 8192), ("w_dn0", 16, 8192), ("w_dn1", 16, 8192), ("w_dn2", 16, 22 * 256)):
        WB[nm] = nc.dram_tensor("wb_" + nm, [ns, 128, ncl], BF).ap()
    converted = {}
    wb_slot = [Slot(nc, "wb0"), Slot(nc, "wb1")]
    slab_slot_hw = [Slot(nc, "slabhw0"), Slot(nc, "slabhw1")]
    wb_pending = [None, None]

    def load_slab(src_ap, ncols):
        i = slab_idx[0] % 2
        slab_idx[0] += 1
        nm = src_ap.name
        key = (nm, src_ap.offset)
        if nm in WB and key in converted:
            wb_ap, wev = converted[key]
            SP.wait(slab_free[i], wb_pending[i], wev)
            ev = dma(SP, slab_slot_hw[i], slabs[i][:, 0:ncols], wb_ap)
            return i, ev
        POOL.wait(slab_free[i], wb_pending[i])
        ev = dma(POOL, slab_slot[i], slabs[i][:, 0:ncols], src_ap)
        if nm in WB:
            idx = src_ap.offset // (128 * ncols)
            wb_ap = WB[nm][idx]
            SP.wait(ev)
            wev = dma(SP, wb_slot[i], wb_ap, slabs[i][:, 0:ncols])
            converted[key] = (wb_ap, wev)
            wb_pending[i] = wev
        return i, ev

    def slab_view(i, kc):
        return slabs[i][:, 0:kc * 256].rearrange("p (k c) -> p k c", c=256)

    WSRC = {"w_in": w_in, "w_out": w_out, "w_cq": w_cq, "w_ckv": w_ckv, "w_co": w_co, "w_up": w_up,
            "w_dn0": w_dn0, "w_dn1": w_dn1, "w_dn2": w_dn2}
    cursor = [0]
    loaded = {}

    def ensure_loaded(g):
        if trace is None or g >= len(trace) or g in loaded:
            return
        nm, off, ncols = trace[g]
        loaded[g] = load_slab(WSRC[nm][off // (128 * ncols)], ncols)

    def stream_slabs(srcs, kcs, body):
        if trace is None:
            nxt = load_slab(srcs[0], kcs[0] * 256)
        for j in range(len(srcs)):
            rec_trace.append((srcs[j].name, srcs[j].offset, kcs[j] * 256))
            if trace is None:
                cur = nxt
                if j + 1 < len(srcs):
                    nxt = load_slab(srcs[j + 1], kcs[j + 1] * 256)
            else:
                g = cursor[0]
                cursor[0] += 1
                assert trace[g] == (srcs[j].name, srcs[j].offset, kcs[j] * 256), "slab trace mismatch"
                ensure_loaded(g)
                ensure_loaded(g + 1)
                cur = loaded.pop(g)
            i, ev = cur
            PE.wait(ev)
            last = body(j, slab_view(i, kcs[j]))
            slab_free[i] = last

    def mm_acc(bank_i, out_ap, pairs, extra_wait=()):
        PE.wait(bank_free[bank_i], *extra_wait)
        n = len(pairs)
        ins = None
        for k, (l, r) in enumerate(pairs):
            ins = PE.e.matmul(out_ap, l, r, start=(k == 0), stop=(k == n - 1))
        return PE.done(ins)

    xnT_free = [None]

    def norm_T(src_rows, nblk, gsel):
        AR.reset()
        xs_b = [xs[:, :], AR.get(4096, F32)]
        xb_b = [xb[:, :], AR.get(4096, BF)]
        xs_free = [None, None]
        xb_free = [None, None]
        for blk in range(nblk):
            k = blk % 2
            c3 = 3 * k
            SP.wait(xs_free[k])
            d = dma(SP, sl_xs2[k], xs_b[k], src_rows(blk))
            ACT.wait(d, xb_free[k])
            a = ACT.done(ACT.e.activation(out=xb_b[k], in_=xs_b[k], func=AF.Square, scale=1.0 / 64.0, accum_out=ss[:, c3:c3 + 1]))
            ACT.wait(a)
            b1 = ACT.done(ACT.e.activation(out=ss[:, c3 + 1:c3 + 2], in_=ss[:, c3:c3 + 1], func=AF.Ln, bias=cst_t[:, 0:1]))
            ACT.wait(b1)
            e1 = ACT.done(ACT.e.activation(out=ss[:, c3 + 2:c3 + 3], in_=ss[:, c3 + 1:c3 + 2], func=AF.Exp, scale=-0.5))
            DVE.wait(e1, xb_free[k])
            e2 = DVE.done(DVE.e.tensor_scalar(out=xb_b[k], in0=xs_b[k], scalar1=ss[:, c3 + 2:c3 + 3], scalar2=None, op0=ALU.mult))
            xs_free[k] = e2
            tr_last = None
            for grp in range(4):
                bi = 6 + (grp % 2)
                pt = banks[bi].bitcast(BF).rearrange("p (a b) -> p a b", b=128)
                PE.wait(e2, bank_free[bi])
                ins = None
                for kk in range(8):
                    c = grp * 8 + kk
                    ins = PE.e.transpose(pt[:, kk, :], xb_b[k][:, c * 128:(c + 1) * 128], ident)
                te = PE.done(ins)
                tr_last = te
                DVE.wait(te, xnT_free[0])
                gb = gcols_t[:, gsel * 32 + grp * 8: gsel * 32 + grp * 8 + 8].unsqueeze(2).to_broadcast([128, 8, 128])
                ee = DVE.done(DVE.e.tensor_tensor(out=xnT_t[:, grp * 8:grp * 8 + 8, blk * 128:(blk + 1) * 128],
                                                  in0=pt[:, 0:8, :], in1=gb, op=ALU.mult))
                bank_free[bi] = ee
            xb_free[k] = tr_last
        barrier()

    def head_norm(src_ap, ntok, gain_ap, out_ap, src_ev, src_is_psum):
        kf = AR.get(ntok, F32)
        sq = AR.get(ntok, BF)
        rs = AR.get(ntok, F32)
        if src_is_psum:
            DVE.wait(src_ev)
            e_kf = DVE.done(DVE.e.tensor_copy(out=kf, in_=src_ap))
            srcf = kf
        else:
            e_kf = src_ev
            srcf = src_ap
        ACT.wait(e_kf)
        e_sq = ACT.done(ACT.e.activation(out=sq, in_=srcf, func=AF.Square))
        b2 = get_bank()
        e_mm = mm_acc(b2, banks[b2][:, 0:ntok], [(ones, sq)], extra_wait=(e_sq,))
        ACT.wait(e_mm)
        r1 = ACT.done(ACT.e.activation(out=rs, in_=banks[b2][:, 0:ntok], func=AF.Ln, bias=cst_t[:, 0:1]))
        bank_free[b2] = r1
        ACT.wait(r1)
        e_r = ACT.done(ACT.e.activation(out=rs, in_=rs, func=AF.Exp, scale=-0.5))
        DVE.wait(e_r, e_kf)
        e_o = DVE.done(DVE.e.scalar_tensor_tensor(out=out_ap, in0=srcf, scalar=gain_ap, in1=rs, op0=ALU.mult, op1=ALU.mult))
        DVE.wait(e_o)
        ACT.wait(e_o)
        return e_o, e_sq, e_kf

    def rope_tok(ps_ap, blk4, out_f32, ev_ps):
        t1 = AR.get(256, F32)
        t2 = AR.get(256, F32)
        p4 = ps_ap.rearrange("p (h two d) -> p h two d", h=2, two=2)
        t14 = t1.rearrange("p (h two d) -> p h two d", h=2, two=2)
        t24 = t2.rearrange("p (h two d) -> p h two d", h=2, two=2)
        cb = rp_c[:, blk4, :].unsqueeze(1).unsqueeze(1).to_broadcast([128, 2, 2, 64])
        sbb = rp_s[:, blk4, :].unsqueeze(1).to_broadcast([128, 2, 64])
        nbb = rp_n[:, blk4, :].unsqueeze(1).to_broadcast([128, 2, 64])
        DVE.wait(ev_ps)
        DVE.e.tensor_tensor(out=t14, in0=p4, in1=cb, op=ALU.mult)
        DVE.e.tensor_tensor(out=t24[:, :, 0, :], in0=p4[:, :, 1, :], in1=nbb, op=ALU.mult)
        e = DVE.done(DVE.e.tensor_tensor(out=t24[:, :, 1, :], in0=p4[:, :, 0, :], in1=sbb, op=ALU.mult))
        DVE.wait(e)
        e2 = DVE.done(DVE.e.tensor_tensor(out=out_f32, in0=t1, in1=t2, op=ALU.add))
        return e2, e

    def load_rope(vt_blk0, nblk):
        for t, src in ((rp_c, ropec), (rp_s, ropes), (rp_n, ropen)):
            s3 = src.rearrange("p (b d) -> p b d", d=64)
            ev = dma(SP, sl_rope, t[:, 0:nblk, :], s3[:, vt_blk0:vt_blk0 + nblk, :])
        return ev

    def mem_kv():
        norm_T(lambda blk: mem[blk * 128:(blk + 1) * 128, :], 2, 3)
        checkpoint("memnorm")
        AR.reset()
        srcs = [w_ckv[i] for i in range(4)]

        def body(j, sv):
            last = None
            if j < 2:
                for hh in range(2):
                    h = 2 * j + hh
                    b = get_bank()
                    e = mm_acc(b, banks[b][:, 0:256], [(sv[:, kc, hh * 128:(hh + 1) * 128], xnT_t[:, kc, 0:256]) for kc in range(32)])
                    last = e
                    checkpoint("mk_mm")
                    eo, esq, ekf = head_norm(banks[b][:, 0:256], 256, hg_t[:, 5:6], kmT[:, h, :], e, True)
                    bank_free[b] = ekf
                    DVE.wait(esq)
                    checkpoint("mk_hn")
            else:
                for blk in range(2):
                    b = get_bank()
                    e = mm_acc(b, banks[b][:, 0:256], [(xnT_t[:, kc, blk * 128:(blk + 1) * 128], sv[:, kc, :]) for kc in range(32)])
                    last = e
                    ACT.wait(e)
                    bank_free[b] = ACT.done(ACT.e.copy(out=vm[:, blk, (j - 2) * 256:(j - 1) * 256], in_=banks[b][:, 0:256]))
            return last
        stream_slabs(srcs, [32] * 4, body)
        xnT_free[0] = (PE.sem, PE.cnt)
        barrier()

    def kv_side(vt):
        AR.reset()
        ev_rope = load_rope(vt * 4, 4)
        DVE.wait(ev_rope)
        kst = [AR.get(512, BF), AR.get(512, BF)]
        kst_free = [None, None]
        vst = [AR.get(4 * 256, BF), AR.get(4 * 256, BF)]
        vst_free = [None, None]
        mark = AR.off
        cnt = [0]
        hn_prev = [None, None]
        srcs = [w_in[40 + i] for i in range(8)] + [w_in[48 + i] for i in range(8)]

        def body(j, sv):
            last = None
            if j < 8:
                AR.off = mark + (j % 2) * 2 * 2560
                ACT.wait(hn_prev[j % 2])
                DVE.wait(hn_prev[j % 2])
                for hh in range(2):
                    h = 2 * j + hh
                    b = get_bank()
                    e = mm_acc(b, banks[b][:, :], [(sv[:, kc, hh * 128:(hh + 1) * 128], xnT_t[:, kc, :]) for kc in range(32)])
                    last = e
                    k = cnt[0] % 2
                    cnt[0] += 1
                    DVE.wait(kst_free[k])
                    eo, esq, ekf = head_norm(banks[b][:, :], 512, hg_t[:, 2:3], kst[k], e, True)
                    bank_free[b] = ekf
                    SP.wait(eo)
                    kst_free[k] = dma(SP, sl_kst[k], KT[h][:, vt * 512:(vt + 1) * 512], kst[k])
                    hn_prev[j % 2] = eo
            else:
                s = j - 8
                k = s % 2
                ACT.wait(vst_free[k])
                ee = None
                for blk in range(4):
                    b = get_bank()
                    e = mm_acc(b, banks[b][:, 0:256], [(xnT_t[:, kc, blk * 128:(blk + 1) * 128], sv[:, kc, :]) for kc in range(32)])
                    last = e
                    ACT.wait(e)
                    ee = ACT.done(ACT.e.copy(out=vst[k][:, blk * 256:(blk + 1) * 256], in_=banks[b][:, 0:256]))
                    bank_free[b] = ee
                SP.wait(ee)
                dst = VS[vt * 512:(vt + 1) * 512, s * 256:(s + 1) * 256].rearrange("(b p) c -> p b c", p=128)
                vst_free[k] = dma(SP, sl_vst[k], dst, vst[k].rearrange("p (b c) -> p b c", c=256))
            return last
        with nc.named_scope("A_sbkv"):
            stream_slabs(srcs, [32] * 16, body)
            barrier()

        AR.reset()
        srcs = []
        for s in range(8):
            srcs += [w_in[8 + s], w_in[16 + s]]
        kd = AR.get(4 * 256, BF)
        kr16 = AR.get(4 * 256, BF)
        vb = AR.get(4 * 256, BF)
        cap = AR.get(256, BF)
        mark2 = AR.off
        hold = {}

        def body2(j, sv):
            s = j // 2
            last = None
            if j % 2 == 0:
                for blk in range(4):
                    AR.off = mark2
                    b = get_bank()
                    e = mm_acc(b, banks[b][:, 0:256], [(xnT_t[:, kc, blk * 128:(blk + 1) * 128], sv[:, kc, :]) for kc in range(32)])
                    last = e
                    krf = AR.get(256, F32)
                    e2, e1 = rope_tok(banks[b][:, 0:256], blk, krf, e)
                    bank_free[b] = e1
                    DVE.wait(e2, hold.get("kdma"))
                    kdb = rtab_t[:, 2 * s:2 * s + 2].unsqueeze(2).to_broadcast([128, 2, 128])
                    DVE.e.tensor_tensor(out=kd[:, blk * 256:(blk + 1) * 256].rearrange("p (h d) -> p h d", h=2),
                                        in0=krf.rearrange("p (h d) -> p h d", h=2), in1=kdb, op=ALU.mult)
                    ek = DVE.done(DVE.e.tensor_copy(out=kr16[:, blk * 256:(blk + 1) * 256], in_=krf))
                    hold[("k", blk)] = ek
                    DVE.wait(ek)
                if vt >= 11:
                    SP.wait(hold[("k", 3)])
                    r0 = vt * 512 - SCR0
                    dst = RKo[r0:r0 + 512, s * 256:(s + 1) * 256].rearrange("(b p) c -> p b c", p=128)
                    hold["kdma"] = dma(SP, sl_rk, dst, kr16.rearrange("p (b c) -> p b c", c=256))
            else:
                for blk in range(4):
                    b = get_bank()
                    e = mm_acc(b, banks[b][:, 0:256], [(xnT_t[:, kc, blk * 128:(blk + 1) * 128], sv[:, kc, :]) for kc in range(32)])
                    last = e
                    ACT.wait(e, hold.get("vdma"), hold.get("kvlast"))
                    ev = ACT.done(ACT.e.copy(out=vb[:, blk * 256:(blk + 1) * 256], in_=banks[b][:, 0:256]))
                    bank_free[b] = ev
                    hold[("v", blk)] = ev
                if vt >= 11:
                    SP.wait(hold[("v", 3)])
                    r0 = vt * 512 - SCR0
                    dst = RVo[r0:r0 + 512, s * 256:(s + 1) * 256].rearrange("(b p) c -> p b c", p=128)
                    hold["vdma"] = dma(SP, sl_rv, dst, vb.rearrange("p (b c) -> p b c", c=256))
                for blk in range(4):
                    n = vt * 4 + blk
                    b = get_bank()
                    PE.wait(bank_free[b], hold[("k", blk)], hold[("v", blk)])
                    ins = None
                    for hh in range(2):
                        ins = PE.e.matmul(banks[b][:, hh * 128:(hh + 1) * 128],
                                          kd[:, blk * 256 + hh * 128: blk * 256 + (hh + 1) * 128],
                                          vb[:, blk * 256 + hh * 128: blk * 256 + (hh + 1) * 128],
                                          start=(hh == 0), stop=(hh == 1), skip_group_check=True)
                    ekv = PE.done(ins)
                    last = ekv
                    st2 = state[:, 2 * s:2 * s + 2, :]
                    if n >= 47:
                        DVE.wait(hold.get("capdma"))
                        ec = DVE.done(DVE.e.tensor_copy(out=cap.rearrange("p (h d) -> p h d", h=2), in_=st2))
                        SP.wait(ec)
                        hold["capdma"] = dma(SP, sl_st, STs[n - 47][:, s * 256:(s + 1) * 256], cap)
                        DVE.wait(ec)
                    db = rtab_t[:, 32 + 2 * s:32 + 2 * s + 2].unsqueeze(2).to_broadcast([128, 2, 128])
                    ed = DVE.done(DVE.e.tensor_tensor(out=st2, in0=st2, in1=db, op=ALU.mult))
                    DVE.wait(ed, ekv)
                    eu = DVE.done(DVE.e.tensor_tensor(out=st2, in0=st2,
                                                      in1=banks[b][:, 0:256].rearrange("p (h d) -> p h d", h=2), op=ALU.add))
                    bank_free[b] = eu
                    DVE.wait(eu)
            return last
        with nc.named_scope("A_ret"):
            stream_slabs(srcs, [32] * 16, body2)
            xnT_free[0] = (PE.sem, PE.cnt)
            barrier()

    def barrier_light():
        es = [PE, ACT, DVE]
        evs = [(e.sem, e.cnt) for e in es if e.cnt > 0]
        for e in es:
            e.wait(*evs)

    def own_pipeline(v0, ntok, c0, out_row0):
        nblk = ntok // 128
        orow = v0 - HALO0
        chunk0 = v0 // 128 - 47
        xq = xnT_t[:, :, c0:c0 + ntok]

        AR.reset()
        ev_rope = load_rope(v0 // 128, nblk)
        DVE.wait(ev_rope)
        qtok = AR.get(4 * 256, BF)
        ktok = AR.get(4 * 256, BF)
        vtok = AR.get(4 * 256, BF)
        sttok = AR.get(4 * 256, BF)
        qT = AR.get(2 * 512, BF)
        kT = AR.get(2 * 512, BF)
        sg = AR.get(2 * 512, F32)
        Sp = AR.get(512, BF)
        of = AR.get(512, F32)
        mark = AR.off
        srcs = []
        for s in range(8):
            srcs += [w_in[0 + s], w_in[24 + s]]
        hold = {}

        def body(j, sv):
            s = j // 2
            last = None
            r0 = v0 - SCR0
            if j % 2 == 0:
                SP.wait(hold.get("ret_done"))
                dma(SP, sl_k[0], ktok[:, 0:nblk * 256].rearrange("p (b c) -> p b c", c=256),
                    RKo[r0:r0 + ntok, s * 256:(s + 1) * 256].rearrange("(b p) c -> p b c", p=128))
                dma(SP, sl_k[0], vtok[:, 0:nblk * 256].rearrange("p (b c) -> p b c", c=256),
                    RVo[r0:r0 + ntok, s * 256:(s + 1) * 256].rearrange("(b p) c -> p b c", p=128))
                dma(SP, sl_k[0], qrow_t[:], qrow[:, s * 256:(s + 1) * 256].rearrange("p (h c) -> p h c", h=2))
                hold["ld"] = dma(SP, sl_k[0], sttok[:, 0:nblk * 256].rearrange("p (b c) -> p b c", c=256),
                                 STs[chunk0:chunk0 + nblk, :, s * 256:(s + 1) * 256].rearrange("b p c -> p b c"))
                for blk in range(nblk):
                    AR.off = mark
                    b = get_bank()
                    e = mm_acc(b, banks[b][:, 0:256], [(xq[:, kc, blk * 128:(blk + 1) * 128], sv[:, kc, :]) for kc in range(32)])
                    last = e
                    qf = AR.get(256, F32)
                    e2, e1 = rope_tok(banks[b][:, 0:256], blk, qf, e)
                    bank_free[b] = e1
                    DVE.wait(e2, hold.get("ret_done"))
                    eq = DVE.done(DVE.e.tensor_copy(out=qtok[:, blk * 256:(blk + 1) * 256], in_=qf))
                    hold[("q", blk)] = eq
                    DVE.wait(eq)
                PE.wait(hold["ld"], hold[("q", nblk - 1)])
                for (src_t, dstT, nm) in ((qtok, qT, "qT"), (ktok, kT, "kT")):
                    bi = 6 if nm == "qT" else 7
                    pt = banks[bi].bitcast(BF).rearrange("p (a b) -> p a b", b=128)
                    PE.wait(bank_free[bi])
                    ins = None
                    for hh in range(2):
                        for blk in range(nblk):
                            ins = PE.e.transpose(pt[:, hh * 4 + blk, :],
                                                 src_t[:, blk * 256 + hh * 128: blk * 256 + (hh + 1) * 128], ident)
                    te = PE.done(ins)
                    last = te
                    ACT.wait(te, hold.get("ret_done"))
                    ee = None
                    for hh in range(2):
                        ee = ACT.e.copy(out=dstT[:, hh * 512: hh * 512 + ntok].rearrange("p (b c) -> p b c", c=128),
                                        in_=pt[:, hh * 4: hh * 4 + nblk, :])
                    ee = ACT.done(ee)
                    bank_free[bi] = ee
                    hold[nm] = ee
            else:
                for hh in range(2):
                    b = get_bank()
                    e = mm_acc(b, banks[b][:, 0:ntok], [(sv[:, kc, hh * 128:(hh + 1) * 128], xq[:, kc, :]) for kc in range(32)])
                    last = e
                    ACT.wait(e, hold.get("ret_done"))
                    eg = ACT.done(ACT.e.activation(out=sg[:, hh * 512: hh * 512 + ntok], in_=banks[b][:, 0:ntok], func=AF.Silu))
                    bank_free[b] = eg
                    hold[("sg", hh)] = eg
                for hh in range(2):
                    h = 2 * s + hh
                    AR.off = mark
                    b = get_bank()
                    PE.wait(bank_free[b], hold["qT"], hold["kT"])
                    ins = None
                    for blk in range(nblk):
                        ins = PE.e.matmul(banks[b][:, blk * 128:(blk + 1) * 128],
                                          kT[:, hh * 512 + blk * 128: hh * 512 + (blk + 1) * 128],
                                          qT[:, hh * 512 + blk * 128: hh * 512 + (blk + 1) * 128],
                                          start=(blk == 0), stop=(blk == nblk - 1), skip_group_check=True)
                    es = PE.done(ins)
                    DVE.wait(es, hold.get(("o", hh ^ 1)), hold.get(("o", hh)))
                    m3 = mask01_f[:, :].unsqueeze(1).to_broadcast([128, nblk, 128])
                    esp = DVE.done(DVE.e.scalar_tensor_tensor(
                        out=Sp[:, 0:ntok].rearrange("p (b c) -> p b c", c=128),
                        in0=banks[b][:, 0:ntok].rearrange("p (b c) -> p b c", c=128),
                        scalar=rtab_t[:, 16 + h:16 + h + 1], in1=m3, op0=ALU.mult, op1=ALU.mult))
                    bank_free[b] = esp
                    b2 = get_bank()
                    PE.wait(bank_free[b2], esp, hold["ld"])
                    ins = None
                    for blk in range(nblk):
                        PE.e.matmul(banks[b2][:, blk * 128:(blk + 1) * 128],
                                    vtok[:, blk * 256 + hh * 128: blk * 256 + (hh + 1) * 128],
                                    Sp[:, blk * 128:(blk + 1) * 128],
                                    start=(blk == 0), stop=False, skip_group_check=True)
                        ins = PE.e.matmul(banks[b2][:, blk * 128:(blk + 1) * 128],
                                          sttok[:, blk * 256 + hh * 128: blk * 256 + (hh + 1) * 128],
                                          qT[:, hh * 512 + blk * 128: hh * 512 + (blk + 1) * 128],
                                          start=False, stop=True, skip_group_check=True)
                    eo = PE.done(ins)
                    last = eo
                    DVE.wait(eo)
                    q3 = qrow_t[:, hh, :].unsqueeze(1).to_broadcast([128, nblk, 128])
                    eof = DVE.done(DVE.e.tensor_tensor(out=of[:, 0:ntok].rearrange("p (b c) -> p b c", c=128),
                                                       in0=banks[b2][:, 0:ntok].rearrange("p (b c) -> p b c", c=128),
                                                       in1=q3, op=ALU.mult))
                    bank_free[b2] = eof
                    on = AR.get(512, F32)
                    eon, esq, ekf = head_norm(of[:, 0:ntok], ntok, hg_t[:, 0:1], on[:, 0:ntok], eof, False)
                    DVE.wait(eon, hold[("sg", hh)])
                    em = DVE.done(DVE.e.tensor_tensor(out=mixT[:, h, 0:ntok], in0=on[:, 0:ntok],
                                                      in1=sg[:, hh * 512: hh * 512 + ntok], op=ALU.mult))
                    hold[("o", hh)] = em
                    DVE.wait(em, esq)
                hold["ret_done"] = (DVE.sem, DVE.cnt)
            return last
        with nc.named_scope("B_ret"):
            stream_slabs(srcs, [32] * 16, body)
            barrier()
        checkpoint(f"ret@{v0}")

        AR.reset()
        qTs = AR.get(2 * 512, BF)
        NKB = 3
        kbuf = [AR.get(2 * 512, BF).rearrange("p (h c) -> p h c", h=2) for _ in range(NKB)]
        vbuf = [AR.get(2 * 512, BF).rearrange("p (h c) -> p h c", h=2) for _ in range(NKB)]
        ebuf = [AR.get(2 * 512, F32).rearrange("p (h c) -> p h c", h=2) for _ in range(2)]
        spb = [AR.get(2 * 512, F16).rearrange("p (h c) -> p h c", h=2) for _ in range(2)]
        gbuf = AR.get(2 * 512, F32).rearrange("p (h c) -> p h c", h=2)
        abuf = AR.get(2 * 512, BF).rearrange("p (h c) -> p h c", h=2)
        of2 = AR.get(2 * 512, F32).rearrange("p (h c) -> p h c", h=2)
        mark3 = AR.off
        srcs = [w_in[32 + s_] for s_ in range(8)]
        kb_max = (v0 + ntok) // 128 - 1
        qblk0 = v0 // 128
        ktiles = list(range(kb_max // 4, -1, -1))
        blocks = []
        for kt in ktiles:
            for kb in range(min(kb_max, kt * 4 + 3), kt * 4 - 1, -1):
                blocks.append((kt, kb))
        nb = len(blocks)
        Zp = [pairs[0][:, :].rearrange("p (h c) -> p h c", h=2), pairs[1][:, :].rearrange("p (h c) -> p h c", h=2)]
        Cp = pairs[2][:, :].rearrange("p (h c) -> p h c", h=2)
        Op = pairs[3][:, :].rearrange("p (h c) -> p h c", h=2)
        hold2 = {}
        kvslot_cnt = [0]

        def body3(j, sv):
            last = None
            qev = []
            for hh in range(2):
                AR.off = mark3
                b = get_bank((0, 1, 2, 3))
                e = mm_acc(b, banks[b][:, 0:ntok], [(sv[:, kc, hh * 128:(hh + 1) * 128], xq[:, kc, :]) for kc in range(32)])
                last = e
                DVE.wait(hold2.get("att_done"))
                eo, esq, ekf = head_norm(banks[b][:, 0:ntok], ntok, hg_t[:, 1:2], qTs[:, hh * 512: hh * 512 + ntok], e, True)
                bank_free[b] = ekf
                qev.append(eo)
                DVE.wait(esq)
            q3 = qTs.rearrange("p (h c) -> p h c", h=2)
            st = {"kvev": {}, "kvfree": [hold2.get("att_done")] * NKB, "Zfree": [None, None], "efree": [None, None], "spfree": [None, None],
                  "gfree": None, "afree": None, "firstC": [True, True], "firstO": [True, True]}
            evQK, evE, evL, evM1, evG, evA, evAV = {}, {}, {}, {}, {}, {}, {}

            def geom(i):
                kt, kb = blocks[i]
                r = kb - qblk0
                col0 = max(r, 0) * 128
                return kt, kb, kb - kt * 4, r, col0

            def load_kv(kt):
                if kt in st["kvev"] or kt < 0:
                    return
                k = kt % NKB
                SP.wait(st["kvfree"][k])
                ev = None
                for hh in range(2):
                    h = 2 * j + hh
                    dma(SP, sl_kv[k], kbuf[k][:, hh, :], KT[h][:, kt * 512:(kt + 1) * 512])
                    ev = dma(SP, sl_kv[k], vbuf[k][:, hh, :].rearrange("p (b c) -> p b c", c=128),
                             VS[kt * 512:(kt + 1) * 512, h * 128:(h + 1) * 128].rearrange("(b p) c -> p b c", p=128))
                st["kvev"][kt] = ev

            def QK(i):
                kt, kb, kl, r, col0 = geom(i)
                load_kv(kt)
                load_kv(kt - 1)
                z = Zp[i % 2]
                PE.wait(st["Zfree"][i % 2], st["kvev"][kt], *qev)
                if i < 2:
                    PE.wait(bank_free[0], bank_free[1], bank_free[2], bank_free[3])
                ins = None
                for hh in range(2):
                    ins = PE.e.matmul(z[:, hh, col0:ntok], kbuf[kt % NKB][:, hh, kl * 128:(kl + 1) * 128], q3[:, hh, col0:ntok],
                                      start=True, stop=True)
                evQK[i] = PE.done(ins)

            def EXP(i):
                kt, kb, kl, r, col0 = geom(i)
                ACT.wait(evQK[i], st["efree"][i % 2])
                evE[i] = ACT.done(ACT.e.activation(out=ebuf[i % 2][:, :, col0:ntok], in_=Zp[i % 2][:, :, col0:ntok], func=AF.Exp, scale=SCALE))
                st["Zfree"][i % 2] = evE[i]

            def LN(i):
                kt, kb, kl, r, col0 = geom(i)
                ACT.wait(evE[i], st["spfree"][i % 2])
                ev = ACT.done(ACT.e.activation(out=spb[i % 2][:, :, col0:ntok], in_=ebuf[i % 2][:, :, col0:ntok], func=AF.Ln, bias=cst_t[:, 1:2]))
                if r >= 0:
                    DVE.wait(ev)
                    mb = dmask_h[:, :].unsqueeze(1).to_broadcast([128, 2, 128])
                    ev = DVE.done(DVE.e.tensor_tensor(out=spb[i % 2][:, :, col0:col0 + 128], in0=spb[i % 2][:, :, col0:col0 + 128],
                                                      in1=mb, op=ALU.mult))
                evL[i] = ev

            def MM1(i):
                kt, kb, kl, r, col0 = geom(i)
                PE.wait(evL[i])
                if i == 0:
                    PE.wait(bank_free[4], bank_free[5], bank_free[6], bank_free[7])
                ins = None
                for hh in range(2):
                    ins = PE.e.matmul(Cp[:, hh, col0:ntok], Umat, spb[i % 2][:, hh, col0:ntok], start=st["firstC"][hh], stop=False,
                                      skip_group_check=True)
                    st["firstC"][hh] = False
                evM1[i] = PE.done(ins)

            def G(i):
                kt, kb, kl, r, col0 = geom(i)
                ACT.wait(evM1[i], st["gfree"])
                evG[i] = ACT.done(ACT.e.activation(out=gbuf[:, :, col0:ntok], in_=Cp[:, :, col0:ntok], func=AF.Exp, scale=-1.0))

            def MM2(i):
                kt, kb, kl, r, col0 = geom(i)
                PE.wait(evG[i])
                ins = None
                for hh in range(2):
                    ins = PE.e.matmul(Cp[:, hh, col0:ntok], Lmat, spb[i % 2][:, hh, col0:ntok], start=False, stop=False,
                                      skip_group_check=True)
                st["spfree"][i % 2] = PE.done(ins)

            def MUL(i):
                kt, kb, kl, r, col0 = geom(i)
                DVE.wait(evG[i], st["afree"])
                ea = DVE.done(DVE.e.tensor_tensor(out=abuf[:, :, col0:ntok], in0=ebuf[i % 2][:, :, col0:ntok], in1=gbuf[:, :, col0:ntok], op=ALU.mult))
                st["gfree"] = ea
                st["efree"][i % 2] = ea
                if r >= 0:
                    DVE.wait(ea)
                    mb = cm_t[:, 384:512].unsqueeze(1).to_broadcast([128, 2, 128])
                    ea = DVE.done(DVE.e.tensor_tensor(out=abuf[:, :, col0:col0 + 128], in0=abuf[:, :, col0:col0 + 128], in1=mb, op=ALU.mult))
                evA[i] = ea

            def AV(i):
                kt, kb, kl, r, col0 = geom(i)
                PE.wait(evA[i])
                ins = None
                for hh in range(2):
                    ins = PE.e.matmul(Op[:, hh, col0:ntok], vbuf[kt % NKB][:, hh, kl * 128:(kl + 1) * 128], abuf[:, hh, col0:ntok],
                                      start=st["firstO"][hh], stop=False, skip_group_check=True)
                    st["firstO"][hh] = False
                evAV[i] = PE.done(ins)
                st["afree"] = evAV[i]
                if i + 1 == nb or blocks[i + 1][0] != kt:
                    st["kvfree"][kt % NKB] = evAV[i]
                    st["kvev"].pop(kt + NKB, None)

            QK(0)
            EXP(0)
            LN(0)
            if nb > 1:
                QK(1)
            for i in range(nb):
                MM1(i)
                if i + 2 < nb:
                    QK(i + 2)
                if i + 1 < nb:
                    EXP(i + 1)
                G(i)
                if i + 1 < nb:
                    LN(i + 1)
                MM2(i)
                MUL(i)
                AV(i)
            last = evAV[nb - 1]
            DVE.wait(last)
            eof = DVE.done(DVE.e.tensor_copy(out=of2[:, :, 0:ntok], in_=Op[:, :, 0:ntok]))
            for bi in range(8):
                bank_free[bi] = eof
            for hh in range(2):
                h = 2 * j + hh
                AR.off = mark3
                eon, esq, ekf = head_norm(of2[:, hh, 0:ntok], ntok, hg_t[:, 3:4], mixT[:, 16 + h, 0:ntok], eof, False)
                DVE.wait(eon, esq)
            barrier_light()
            hold2["att_done"] = (DVE.sem, DVE.cnt)
            return last
        with nc.named_scope("B_sb"):
            stream_slabs(srcs, [32] * 8, body3)
            barrier()
        checkpoint(f"sb@{v0}")

        def proj_residual(wsrc, nslab, kcs, lhs_of, res_rows, dst_rows, final=False):
            rbuf = [AR.get(4 * 256, F32), AR.get(4 * 256, F32)]
            rfree = [None, None]
            cnt2 = [0]

            def bodyp(j, sv):
                k = cnt2[0] % 2
                cnt2[0] += 1
                SP.wait(rfree[k])
                eld = dma(SP, sl_rld[k], rbuf[k][:, 0:nblk * 256].rearrange("p (b c) -> p b c", c=256),
                          res_rows[:, j * 256:(j + 1) * 256].rearrange("(b p) c -> p b c", p=128))
                last = None
                ee = None
                for blk in range(nblk):
                    b = get_bank()
                    e = mm_acc(b, banks[b][:, 0:256], [(lhs_of(kc, blk), sv[:, kc, :]) for kc in range(kcs)])
                    last = e
                    DVE.wait(e, eld)
                    ee = DVE.done(DVE.e.tensor_tensor(out=rbuf[k][:, blk * 256:(blk + 1) * 256], in0=rbuf[k][:, blk * 256:(blk + 1) * 256],
                                                      in1=banks[b][:, 0:256], op=ALU.add))
                    bank_free[b] = ee
                SP.wait(ee)
                rfree[k] = dma(SP, sl_rst[k], dst_rows[:, j * 256:(j + 1) * 256].rearrange("(b p) c -> p b c", p=128),
                               rbuf[k][:, 0:nblk * 256].rearrange("p (b c) -> p b c", c=256))
                return last
            stream_slabs([wsrc[i] for i in range(nslab)], [kcs] * nslab, bodyp)
            barrier()

        xrows = x[v0:v0 + ntok, :]
        AR.reset()
        with nc.named_scope("B_wout"):
            proj_residual(w_out, 16, 32, lambda kc, blk: mixT[:, kc, blk * 128:(blk + 1) * 128], xrows, H1[orow:orow + ntok, :])
        checkpoint(f"h1@{v0}")

        norm_T(lambda blk: H1[orow + blk * 128: orow + (blk + 1) * 128, :], nblk, 1)
        AR.reset()
        qc = AR.get(4 * 512, BF)
        oc = AR.get(4 * 512, BF)
        pbuf = AR.get(256, F32)
        pb16 = AR.get(256, BF)
        pT = AR.get(256, BF)
        mark4 = AR.off

        def bodyq(j, sv):
            last = None
            for hh in range(2):
                h = 2 * j + hh
                AR.off = mark4
                b = get_bank()
                e = mm_acc(b, banks[b][:, 0:ntok], [(sv[:, kc, hh * 128:(hh + 1) * 128], xnT_t[:, kc, 0:ntok]) for kc in range(32)])
                last = e
                eo, esq, ekf = head_norm(banks[b][:, 0:ntok], ntok, hg_t[:, 4:5], qc[:, h * 512: h * 512 + ntok], e, True)
                bank_free[b] = ekf
                DVE.wait(eo, esq)
                barrier_light()
            return last
        stream_slabs([w_cq[0], w_cq[1]], [32, 32], bodyq)
        xnT_free[0] = (PE.sem, PE.cnt)
        barrier()
        xprev = {}
        for h in range(4):
            for blk in range(nblk):
                b = get_bank()
                e = mm_acc(b, banks[b][:, 0:256], [(qc[:, h * 512 + blk * 128: h * 512 + (blk + 1) * 128], kmT[:, h, :])])
                DVE.wait(e)
                e0 = DVE.done(DVE.e.tensor_reduce(out=ss[:, 4:5], in_=banks[b][:, 0:256], axis=AX.X, op=ALU.max))
                DVE.wait(e0)
                e1 = DVE.done(DVE.e.tensor_scalar(out=ss[:, 5:6], in0=ss[:, 4:5], scalar1=-SCALE, scalar2=None, op0=ALU.mult))
                ACT.wait(e1, xprev.get("e4"))
                e2 = ACT.done(ACT.e.activation(out=pbuf, in_=banks[b][:, 0:256], func=AF.Exp, bias=ss[:, 5:6], scale=SCALE,
                                               accum_out=ss[:, 6:7]))
                bank_free[b] = e2
                DVE.wait(e2)
                e3 = DVE.done(DVE.e.reciprocal(out=ss[:, 7:8], in_=ss[:, 6:7]))
                DVE.wait(e3, xprev.get("te"))
                e4 = DVE.done(DVE.e.tensor_scalar(out=pb16, in0=pbuf, scalar1=ss[:, 7:8], scalar2=None, op0=ALU.mult))
                xprev["e4"] = e4
                pt = banks[6].bitcast(BF).rearrange("p (a b) -> p a b", b=128)
                PE.wait(e4, bank_free[6])
                PE.e.transpose(pt[:, 0, :], pb16[:, 0:128], ident)
                te = PE.done(PE.e.transpose(pt[:, 1, :], pb16[:, 128:256], ident))
                xprev["te"] = te
                ACT.wait(te)
                e5 = ACT.done(ACT.e.copy(out=pT.rearrange("p (a b) -> p a b", b=128), in_=pt[:, 0:2, :]))
                bank_free[6] = e5
                b2 = get_bank()
                e6 = mm_acc(b2, banks[b2][:, 0:128], [(vm[:, mb, h * 128:(h + 1) * 128], pT[:, mb * 128:(mb + 1) * 128]) for mb in range(2)],
                            extra_wait=(e5,))
                ACT.wait(e6)
                e7 = ACT.done(ACT.e.copy(out=oc[:, h * 512 + blk * 128: h * 512 + (blk + 1) * 128], in_=banks[b2][:, 0:128]))
                bank_free[b2] = e7
        barrier()
        proj_residual(w_co, 16, 4, lambda kc, blk: oc[:, kc * 512 + blk * 128: kc * 512 + (blk + 1) * 128],
                      H1[orow:orow + ntok, :], H2[orow:orow + ntok, :])

        checkpoint(f"h2@{v0}")
        norm_T(lambda blk: H2[orow + blk * 128: orow + (blk + 1) * 128, :], nblk, 2)
        is_halo = (ntok == 128)
        AR.reset()
        ARF = [sb_ff[0], sb_ff[1]]

        def bodyu(j, sv):
            last = None
            cs = []
            for gv in range(2):
                ub = ARF[gv]
                b = get_bank()
                e = mm_acc(b, banks[b][:, 0:ntok], [(sv[:, kc, gv * 128:(gv + 1) * 128], xnT_t[:, kc, 0:ntok]) for kc in range(32)])
                last = e
                ACT.wait(e, fhold.get(("c", gv)))
                ACT.e.copy(out=ub[:, 0:2], in_=carry[:, j * 2 + gv, :])
                eu = ACT.done(ACT.e.copy(out=ub[:, 2:2 + ntok], in_=banks[b][:, 0:ntok]))
                bank_free[b] = eu
                DVE.wait(eu, fhold.get("mul"))
                base = (j * 2 + gv) * 4
                cb = cbuf[gv]
                if not is_halo:
                    ea = DVE.done(DVE.e.tensor_scalar(out=cb[:, 0:ntok], in0=ub[:, 2:2 + ntok], scalar1=cw_t[:, base + 2:base + 3],
                                        scalar2=cw_t[:, base + 3:base + 4], op0=ALU.mult, op1=ALU.add))
                    DVE.wait(ea)
                    ea = DVE.done(DVE.e.scalar_tensor_tensor(out=cb[:, 0:ntok], in0=ub[:, 1:1 + ntok], scalar=cw_t[:, base + 1:base + 2],
                                               in1=cb[:, 0:ntok], op0=ALU.mult, op1=ALU.add))
                    DVE.wait(ea)
                    ec = DVE.done(DVE.e.scalar_tensor_tensor(out=cb[:, 0:ntok], in0=ub[:, 0:ntok], scalar=cw_t[:, base:base + 1],
                                                             in1=cb[:, 0:ntok], op0=ALU.mult, op1=ALU.add))
                    cs.append(ec)
                ek = DVE.done(DVE.e.tensor_copy(out=carry[:, j * 2 + gv, :], in_=ub[:, ntok:ntok + 2]))
                fhold[("c", gv)] = ek
            if not is_halo:
                ACT.wait(cs[0])
                es = ACT.done(ACT.e.activation(out=cbuf[0][:, 0:ntok], in_=cbuf[0][:, 0:ntok], func=AF.Silu))
                DVE.wait(es, cs[1])
                em = DVE.done(DVE.e.tensor_tensor(out=aT[:, j, 0:ntok], in0=cbuf[0][:, 0:ntok], in1=cbuf[1][:, 0:ntok], op=ALU.mult))
                fhold["mul"] = em
            return last
        fhold = {}
        with nc.named_scope("B_up"):
            stream_slabs([w_up[i] for i in range(NFC)], [32] * NFC, bodyu)
        xnT_free[0] = (PE.sem, PE.cnt)
        barrier()
        checkpoint(f"up@{v0}")
        if is_halo:
            DVE.done(DVE.e.tensor_scalar(out=carry[:], in0=carry[:], scalar1=flag_t[:, 0:1], scalar2=None, op0=ALU.mult))
            barrier()
            return
        srcs = []
        kcs = []
        for cg in range(16):
            srcs += [w_dn0[cg], w_dn1[cg], w_dn2[cg]]
            kcs += [32, 32, 22]
        dbank = [[0, 1, 2, 3], [4, 5, 0, 1]]
        rb = [fbuf[0], fbuf[1]]
        rfree = [None, None]
        dst_rows = y[out_row0:out_row0 + ntok, :]
        res_rows = H2[orow:orow + ntok, :]
        dstate = {}

        def bodyd(j, sv):
            cg, kg = j // 3, j % 3
            k = cg % 2
            bs = [0, 1, 2, 3] if k == 0 else [4, 5, 6, 7]
            if kg == 0:
                SP.wait(rfree[k])
                dstate["ld"] = dma(SP, sl_rld[k], rb[k].rearrange("p (b c) -> p b c", c=256),
                                   res_rows[:, cg * 256:(cg + 1) * 256].rearrange("(b p) c -> p b c", p=128))
            last = None
            koff = kg * 32
            nk = kcs[j]
            for blk in range(4):
                b = bs[blk]
                if kg == 0:
                    PE.wait(bank_free[b])
                ins = None
                for kc in range(nk):
                    ins = PE.e.matmul(banks[b][:, 0:256], aT[:, koff + kc, blk * 128:(blk + 1) * 128], sv[:, kc, :],
                                      start=(kg == 0 and kc == 0), stop=(kg == 2 and kc == nk - 1))
                e = PE.done(ins)
                last = e
                if kg == 2:
                    DVE.wait(e, dstate["ld"])
                    ee = DVE.done(DVE.e.tensor_tensor(out=rb[k][:, blk * 256:(blk + 1) * 256], in0=rb[k][:, blk * 256:(blk + 1) * 256],
                                                      in1=banks[b][:, 0:256], op=ALU.add))
                    bank_free[b] = ee
                    dstate["ee"] = ee
            if kg == 2:
                SP.wait(dstate["ee"])
                rfree[k] = dma(SP, sl_rst[k], dst_rows[:, cg * 256:(cg + 1) * 256].rearrange("(b p) c -> p b c", p=128),
                               rb[k].rearrange("p (b c) -> p b c", c=256))
            return last
        with nc.named_scope("B_down"):
            stream_slabs(srcs, kcs, bodyd)
            barrier()

    sb_ff = [xs[:, 0:516], xs[:, 520:1036]]
    cbuf = [xs[:, 1040:1552], xs[:, 1552:2064]]
    fbuf = [xs[:, 2064:3088], xb[:, 0:2048].bitcast(F32)]

    try:
        checkpoint("const")
        mem_kv()
        checkpoint("mem")
        for vt in range(16):
            with nc.named_scope("A_norm"):
                norm_T(lambda blk, vt=vt: x[vt * 512 + blk * 128: vt * 512 + (blk + 1) * 128, :], 4, 0)
            checkpoint(f"norm{vt}")
            kv_side(vt)
            checkpoint(f"kv{vt}")
            if vt == 11:
                own_pipeline(HALO0, 128, 384, 0)
                checkpoint("halo")
            if vt >= 12:
                own_pipeline(vt * 512, 512, 0, (vt - 12) * 512)
                checkpoint(f"own{vt}")
    except _Stop:
        pass
    barrier()
    if debug:
        dbg = nc.dram_tensor("dbg_xnT", [128, 32 * 512], BF, kind="ExternalOutput").ap()
        ev = dma(SP, sl_dbg, dbg, xnT_t[:].rearrange("p c t -> p (c t)"))
        dbg2 = nc.dram_tensor("dbg_km", [128, 4 * 256 + 2 * 512], BF, kind="ExternalOutput").ap()
        dma(SP, sl_dbg, dbg2[:, 0:1024], kmT[:].rearrange("p c t -> p (c t)"))
        ev = dma(SP, sl_dbg, dbg2[:, 1024:2048], vm[:].rearrange("p c t -> p (c t)"))
        SP.wait(ev)
    nc._slab_trace = rec_trace
    return nc


def _pack(W):
    K, N = W.shape
    KC, NS = K // 128, N // 256
    return np.ascontiguousarray(W.reshape(KC, 128, NS, 256).transpose(2, 1, 0, 3)).reshape(NS, 128, KC * 256)


_CACHE = {}


def kernel(x, mem, attn_norm, w_in, ret_norm, sb_q_norm, sb_k_norm, sb_out_norm, w_out,
           cross_norm, mem_norm, cross_w_q, cross_w_kv, cross_q_norm, cross_k_norm, cross_w_o,
           ffn_norm, ffn_w_up, ffn_conv_w, ffn_conv_b, ffn_w_down):
    f32 = np.float32
    x = np.asarray(x, f32)
    mem = np.asarray(mem, f32)
    common = {}
    common["w_in"] = _pack(np.asarray(w_in[0], f32))
    common["w_out"] = _pack(np.asarray(w_out[0], f32))
    common["w_cq"] = _pack(np.asarray(cross_w_q[0], f32))
    common["w_ckv"] = _pack(np.asarray(cross_w_kv[0], f32))
    common["w_co"] = _pack(np.asarray(cross_w_o[0], f32))
    wu = np.asarray(ffn_w_up[0], f32)
    wup = np.empty((D, NFC, 2, 128), f32)
    wup[:, :, 0, :] = wu[:, :DFF].reshape(D, NFC, 128)
    wup[:, :, 1, :] = wu[:, DFF:].reshape(D, NFC, 128)
    common["w_up"] = _pack(wup.reshape(D, NFC * 256))
    del wup
    wd = np.asarray(ffn_w_down[0], f32)
    common["w_dn0"] = _pack(wd[0:4096])
    common["w_dn1"] = _pack(wd[4096:8192])
    common["w_dn2"] = _pack(wd[8192:DFF])

    def fcol(g):
        return np.asarray(g, f32).reshape(32, 128).T
    common["gcols"] = np.ascontiguousarray(np.concatenate(
        [fcol(attn_norm[0]), fcol(cross_norm[0]), fcol(ffn_norm[0]), fcol(mem_norm[0])], axis=1))
    hgm = np.zeros((128, 8), f32)
    for i, g in enumerate((ret_norm, sb_q_norm, sb_k_norm, sb_out_norm, cross_q_norm, cross_k_norm)):
        hgm[:, i] = np.asarray(g[0], f32)
    common["hg"] = hgm
    cwv = np.asarray(ffn_conv_w[0], f32)
    cbv = np.asarray(ffn_conv_b[0], f32)
    cwt = np.zeros((128, NFC, 2, 4), f32)
    for gv in range(2):
        for t in range(3):
            cwt[:, :, gv, t] = cwv[t, gv * DFF:(gv + 1) * DFF].reshape(NFC, 128).T
        cwt[:, :, gv, 3] = cbv[gv * DFF:(gv + 1) * DFF].reshape(NFC, 128).T
    common["cw"] = cwt.reshape(128, NFC * 8)
    hh = np.arange(NH, dtype=np.float64)
    log_g = np.log1p(-np.exp2(-5.0 - hh))
    p = np.arange(128, dtype=np.float64)
    rt = np.zeros((128, 48), f32)
    rt[:, 0:16] = np.exp((127 - p)[:, None] * log_g[None, :])
    rt[:, 16:32] = np.exp(-(p + 1)[:, None] * log_g[None, :])
    rt[:, 32:48] = np.exp(128 * log_g)[None, :]
    common["rtab"] = rt
    qr = (np.exp((p + 1)[None, :] * log_g[:, None]) * SCALE).astype(f32)
    common["qrow"] = np.ascontiguousarray(np.broadcast_to(qr.reshape(1, NH * 128), (128, NH * 128)))
    cm = np.zeros((128, 640), f32)
    cm[:, 0:128] = np.eye(128)
    cm[:, 128:256] = 1.0 / 128.0
    pi = np.arange(128)
    cm[:, 256:384] = (pi[:, None] <= pi[None, :])
    cm[:, 384:512] = (pi[:, None] < pi[None, :])
    common["cmats"] = cm.astype(ml_dtypes.bfloat16)
    hm = np.zeros((128, 256), np.float16)
    hm[:, 0:128] = (pi[:, None] >= pi[None, :])
    hm[:, 128:256] = (pi[:, None] < pi[None, :])
    common["hmats"] = hm

    inv_freq = (10000.0 ** (-np.linspace(0.0, 1.0, 64, dtype=f32))).astype(f32)
    in_maps = []
    for c in range(8):
        b, q = c // 4, c % 4
        shift = (3 - q) * 2048
        xp = np.zeros((S, D), f32)
        xp[shift:] = x[b, :S - shift]
        pos = (np.arange(S) - shift).astype(f32)
        pos = np.maximum(pos, 0).astype(f32)
        ang = (pos[:, None] * inv_freq[None, :]).astype(f32)
        co = np.cos(ang.astype(np.float64)).astype(f32).reshape(64, 128, 64).transpose(1, 0, 2).reshape(128, 4096)
        si = np.sin(ang.astype(np.float64)).astype(f32).reshape(64, 128, 64).transpose(1, 0, 2).reshape(128, 4096)
        m = dict(common)
        m["x"] = xp
        m["mem"] = np.ascontiguousarray(mem[b])
        m["ropec"] = np.ascontiguousarray(co)
        m["ropes"] = np.ascontiguousarray(si)
        m["ropen"] = np.ascontiguousarray(-si)
        m["flag"] = np.full((128, 1), 0.0 if q == 0 else 1.0, f32)
        in_maps.append(m)
    if _CACHE.get("prep_only"):
        return in_maps
    tr = build_program()._slab_trace
    nc = build_program(trace=tr)
    res = run_bass_kernel_spmd(nc, in_maps, core_ids=list(range(8)))
    out = np.zeros((2, S, D), f32)
    for c in range(8):
        b, q = c // 4, c % 4
        out[b, q * 2048:(q + 1) * 2048] = np.asarray(res.results[c]["y"], f32)
    return out
```
